# Optimizing a Trainium2 kernel written in Bass

```python
import jax
import jax.numpy as jnp
from jax import lax
import numpy as np

D_MODEL = 1024
BATCH = 16
SEQ = 4096
DEPTH = 2

CHUNK = 64
Q_BLOCK = 128
HEAD_DIM = 128
MIX_WIDTH = D_MODEL // 2
N_HEADS = MIX_WIDTH // HEAD_DIM
N_BRANCHES = 3
N_GROUPS = 4
EXPERTS_PER_GROUP = 8
N_EXPERTS = N_GROUPS * EXPERTS_PER_GROUP
TOP_K = 2
D_FF_EXPERT = D_MODEL // 2
MOE_BLOCK = 128
DEEPNORM_ALPHA = (2 * DEPTH) ** 0.25
DEEPNORM_BETA = (8 * DEPTH) ** -0.25
LN_EPS = 1e-5
HEAD_NORM_EPS = 1e-6
RET_ROPE_BASE = 10000.0
IN_SIZES = (MIX_WIDTH,) * 4 + (MIX_WIDTH,) * 3 + (N_HEADS,) + (MIX_WIDTH,) * 4 + (D_MODEL,) * N_BRANCHES
D_IN = sum(IN_SIZES)

kernel_name = "hybrid_hgrn2_fox_retnet_hmoe_deepnorm"


def layer_norm(x, g, b):
    xf = x.astype(jnp.float32)
    mu = xf.mean(-1, keepdims=True)
    var = jnp.square(xf - mu).mean(-1, keepdims=True)
    return ((xf - mu) * lax.rsqrt(var + LN_EPS) * g + b).astype(x.dtype)


def head_rms_norm(t):
    return t * lax.rsqrt(jnp.mean(jnp.square(t), -1, keepdims=True) + HEAD_NORM_EPS)


def head_layer_norm(t):
    mu = t.mean(-1, keepdims=True)
    c = t - mu
    return c * lax.rsqrt(jnp.mean(jnp.square(c), -1, keepdims=True) + HEAD_NORM_EPS)


def to_chunks(t):
    b, s, h, d = t.shape
    return t.reshape(b, s // CHUNK, CHUNK, h, d).transpose(1, 0, 3, 2, 4)


def from_chunks(t):
    n, b, h, c, d = t.shape
    return t.transpose(1, 0, 3, 2, 4).reshape(b, n * c, h, d)


def rope(t, pos):
    half = t.shape[-1] // 2
    inv = 1.0 / (RET_ROPE_BASE ** jnp.linspace(0.0, 1.0, half, dtype=jnp.float32))
    ang = pos[:, None] * inv[None, :]
    cos = jnp.cos(ang)[None, :, None, :]
    sin = jnp.sin(ang)[None, :, None, :]
    t1, t2 = t[..., :half], t[..., half:]
    return jnp.concatenate([t1 * cos - t2 * sin, t1 * sin + t2 * cos], axis=-1)


def hgrn2_chunk_scan(q, k, v, log_f):
    bsz, _, h, dk = q.shape
    dv = v.shape[-1]
    causal = jnp.tril(jnp.ones((CHUNK, CHUNK), dtype=bool))[:, :, None]

    def step(state, xs):
        qc, kc, vc, lfc = xs
        cum = jnp.cumsum(lfc, axis=2)
        diff = cum[:, :, :, None, :] - cum[:, :, None, :, :]
        decay = jnp.where(causal, jnp.exp(jnp.where(causal, diff, 0.0)), 0.0)
        scores = jnp.einsum('bhtd,bhsd,bhtsd->bhts', qc, kc, decay)
        o = (jnp.einsum('bhts,bhsv->bhtv', scores, vc)
             + jnp.einsum('bhtd,bhdv->bhtv', qc * jnp.exp(cum), state))
        last = cum[:, :, -1:, :]
        state = (jnp.exp(last[:, :, 0, :])[..., None] * state
                 + jnp.einsum('bhsd,bhsv->bhdv', kc * jnp.exp(last - cum), vc))
        return state, o

    s0 = jnp.zeros((bsz, h, dk, dv), jnp.float32)
    _, o = lax.scan(step, s0, (to_chunks(q), to_chunks(k), to_chunks(v), to_chunks(log_f)))
    return from_chunks(o)


def retention_chunk_scan(q, k, v, log_gamma):
    bsz, _, h, dk = q.shape
    dv = v.shape[-1]
    idx = jnp.arange(CHUNK, dtype=jnp.float32)
    rel = idx[:, None] - idx[None, :]
    decay_mask = jnp.where(rel >= 0, jnp.exp(log_gamma[:, None, None] * jnp.maximum(rel, 0.0)), 0.0)
    q_decay = jnp.exp(log_gamma[:, None] * (idx + 1.0))[..., None]
    k_decay = jnp.exp(log_gamma[:, None] * (CHUNK - 1.0 - idx))[..., None]
    chunk_decay = jnp.exp(log_gamma * CHUNK)[:, None, None]

    def step(state, xs):
        qc, kc, vc = xs
        inner = jnp.einsum('bhtd,bhsd->bhts', qc, kc) * decay_mask
        o = (jnp.einsum('bhts,bhsv->bhtv', inner, vc)
             + jnp.einsum('bhtd,bhdv->bhtv', qc * q_decay, state))
        state = chunk_decay * state + jnp.einsum('bhsd,bhsv->bhdv', kc * k_decay, vc)
        return state, o

    s0 = jnp.zeros((bsz, h, dk, dv), jnp.float32)
    _, o = lax.scan(step, s0, (to_chunks(q), to_chunks(k), to_chunks(v)))
    return from_chunks(o)


def forgetting_attention(q, k, v, log_f):
    bsz, seq, h, dh = q.shape
    n_blocks = seq // Q_BLOCK
    cum = jnp.cumsum(log_f, axis=1).transpose(0, 2, 1)
    kh = k.transpose(0, 2, 1, 3)
    vh = v.transpose(0, 2, 1, 3)
    q_blocks = q.reshape(bsz, n_blocks, Q_BLOCK, h, dh).transpose(1, 0, 3, 2, 4)
    cum_blocks = cum.reshape(bsz, h, n_blocks, Q_BLOCK).transpose(2, 0, 1, 3)
    key_pos = jnp.arange(seq)
    scale = dh ** -0.5

    def block(args):
        qb, cum_q, blk = args
        logits = (jnp.einsum('bhqd,bhkd->bhqk', qb, kh) * scale
                  + cum_q[..., None] - cum[:, :, None, :])
        q_pos = blk * Q_BLOCK + jnp.arange(Q_BLOCK)
        logits = jnp.where(key_pos[None, :] <= q_pos[:, None], logits, -jnp.inf)
        return jnp.einsum('bhqk,bhkd->bhqd', jax.nn.softmax(logits, axis=-1), vh)

    o = lax.map(block, (q_blocks, cum_blocks, jnp.arange(n_blocks)))
    return o.transpose(1, 0, 3, 2, 4).reshape(bsz, seq, h, dh)


def token_mixer(x, w_in, w_branch, w_out, fox_bias, lower_bound):
    bsz, seq, _ = x.shape
    f32 = jnp.float32
    split_at = np.cumsum(IN_SIZES)[:-1].tolist()
    (a_q, a_f, a_i, a_g, b_q, b_k, b_v, b_f, c_q, c_k, c_v, c_g,
     g_a, g_b, g_c) = jnp.split(x @ w_in, split_at, axis=-1)

    def heads(t):
        return t.reshape(bsz, seq, N_HEADS, -1).astype(f32)

    log_fa = jnp.logaddexp(jnp.log(lower_bound), jnp.log1p(-lower_bound) + jax.nn.log_sigmoid(a_f.astype(f32)))
    key_a = -jnp.expm1(log_fa)
    o_a = hgrn2_chunk_scan(heads(a_q), heads(key_a), heads(a_i), heads(log_fa))
    y_a = head_rms_norm(o_a) * jax.nn.sigmoid(heads(a_g))

    log_fb = jax.nn.log_sigmoid(b_f.astype(f32) + fox_bias.astype(f32))
    y_b = forgetting_attention(heads(b_q), heads(b_k), heads(b_v), log_fb)

    pos = jnp.arange(seq, dtype=f32)
    log_gamma = jnp.log(1.0 - jnp.power(2.0, -5.0 - jnp.arange(N_HEADS, dtype=f32)))
    o_c = retention_chunk_scan(rope(heads(c_q), pos), rope(heads(c_k), pos) * HEAD_DIM ** -0.5,
                               heads(c_v), log_gamma)
    y_c = head_layer_norm(o_c) * jax.nn.silu(heads(c_g))

    def flat(t):
        return t.reshape(bsz, seq, MIX_WIDTH).astype(x.dtype)

    merged = (jax.nn.sigmoid(g_a) * (flat(y_a) @ w_branch[0])
              + jax.nn.sigmoid(g_b) * (flat(y_b) @ w_branch[1])
              + jax.nn.sigmoid(g_c) * (flat(y_c) @ w_branch[2]))
    return merged @ w_out


def hierarchical_moe(x, w_rg, w_re, w_up, w_gate, w_down):
    bsz, seq, d = x.shape
    n_tok = bsz * seq
    xf = x.reshape(n_tok, d)
    group_logits = (xf @ w_rg).astype(jnp.float32)
    group_idx = jnp.argmax(group_logits, axis=-1)
    group_prob = jnp.take_along_axis(jax.nn.softmax(group_logits, axis=-1), group_idx[:, None], axis=-1)
    expert_logits = (xf @ w_re).astype(jnp.float32).reshape(n_tok, N_GROUPS, EXPERTS_PER_GROUP)
    in_group = jnp.take_along_axis(expert_logits, group_idx[:, None, None], axis=1)[:, 0]
    top_val, top_idx = lax.top_k(in_group, TOP_K)
    gate = (group_prob * jax.nn.softmax(top_val, axis=-1)).reshape(-1).astype(x.dtype)
    expert_id = (group_idx[:, None] * EXPERTS_PER_GROUP + top_idx).reshape(-1)
    token_id = jnp.repeat(jnp.arange(n_tok, dtype=jnp.int32), TOP_K)

    n_assign = n_tok * TOP_K
    order = jnp.argsort(expert_id)
    sorted_e = expert_id[order]
    counts = jnp.bincount(expert_id, length=N_EXPERTS)
    starts = jnp.cumsum(counts) - counts
    padded = (counts + MOE_BLOCK - 1) // MOE_BLOCK * MOE_BLOCK
    padded_end = jnp.cumsum(padded)
    padded_start = padded_end - padded
    dest = padded_start[sorted_e] + jnp.arange(n_assign) - starts[sorted_e]
    n_rows = n_assign + N_EXPERTS * MOE_BLOCK
    n_blocks = n_rows // MOE_BLOCK
    row_token = jnp.zeros((n_rows,), jnp.int32).at[dest].set(token_id[order])
    row_gate = jnp.zeros((n_rows,), x.dtype).at[dest].set(gate[order])
    block_start = jnp.arange(n_blocks) * MOE_BLOCK
    block_expert = jnp.minimum(jnp.sum(block_start[:, None] >= padded_end[None, :], axis=1), N_EXPERTS - 1)
    x_rows = xf[row_token].reshape(n_blocks, MOE_BLOCK, d)

    def expert_block(args):
        xb, e = args
        h = jax.nn.silu(xb @ w_gate[e]) * (xb @ w_up[e])
        return h @ w_down[e]

    y_rows = lax.map(expert_block, (x_rows, block_expert)).reshape(n_rows, d)
    y = jax.ops.segment_sum(y_rows * row_gate[:, None], row_token, num_segments=n_tok)
    return y.reshape(bsz, seq, d)


def setup_inputs(seed: int = 0) -> dict:
    key = jax.random.key(seed)
    ks = jax.random.split(key, 16)
    nrm = jax.random.normal
    f32 = jnp.float32
    return {
        "x": nrm(ks[0], (BATCH, SEQ, D_MODEL), f32),
        "w_in": nrm(ks[1], (DEPTH, D_MODEL, D_IN), f32) * D_MODEL ** -0.5,
        "w_branch": nrm(ks[2], (DEPTH, N_BRANCHES, MIX_WIDTH, D_MODEL), f32) * MIX_WIDTH ** -0.5,
        "w_out": nrm(ks[3], (DEPTH, D_MODEL, D_MODEL), f32) * (D_MODEL ** -0.5 * DEEPNORM_BETA),
        "fox_fgate_bias": 0.01 * nrm(ks[4], (DEPTH, N_HEADS), f32),
        "hgrn_lb_logits": 0.1 * nrm(ks[5], (DEPTH, MIX_WIDTH), f32),
        "ln1_g": 1.0 + 0.01 * nrm(ks[6], (DEPTH, D_MODEL), f32),
        "ln1_b": 0.01 * nrm(ks[7], (DEPTH, D_MODEL), f32),
        "w_router_group": nrm(ks[8], (DEPTH, D_MODEL, N_GROUPS), f32) * D_MODEL ** -0.5,
        "w_router_expert": nrm(ks[9], (DEPTH, D_MODEL, N_EXPERTS), f32) * D_MODEL ** -0.5,
        "w_up": nrm(ks[10], (DEPTH, N_EXPERTS, D_MODEL, D_FF_EXPERT), f32) * D_MODEL ** -0.5,
        "w_gate": nrm(ks[11], (DEPTH, N_EXPERTS, D_MODEL, D_FF_EXPERT), f32) * D_MODEL ** -0.5,
        "w_down": nrm(ks[12], (DEPTH, N_EXPERTS, D_FF_EXPERT, D_MODEL), f32) * (D_FF_EXPERT ** -0.5 * DEEPNORM_BETA),
        "ln2_g": 1.0 + 0.01 * nrm(ks[13], (DEPTH, D_MODEL), f32),
        "ln2_b": 0.01 * nrm(ks[14], (DEPTH, D_MODEL), f32),
    }


def reference(x, w_in, w_branch, w_out, fox_fgate_bias, hgrn_lb_logits, ln1_g, ln1_b,
              w_router_group, w_router_expert, w_up, w_gate, w_down, ln2_g, ln2_b):
    lb_cum = jnp.cumsum(jax.nn.softmax(hgrn_lb_logits.astype(jnp.float32), axis=0), axis=0)
    lower_bounds = lb_cum - lb_cum[0]
    for layer in range(DEPTH):
        mix = token_mixer(x, w_in[layer], w_branch[layer], w_out[layer],
                          fox_fgate_bias[layer], lower_bounds[layer])
        x = layer_norm(DEEPNORM_ALPHA * x + mix, ln1_g[layer], ln1_b[layer])
        ffn = hierarchical_moe(x, w_router_group[layer], w_router_expert[layer],
                               w_up[layer], w_gate[layer], w_down[layer])
        x = layer_norm(DEEPNORM_ALPHA * x + ffn, ln2_g[layer], ln2_b[layer])
    return x
```

```python
import numpy as np
import concourse.bass as bass
import concourse.mybir as mybir
from concourse.bass_utils import run_bass_kernel_spmd

F32 = mybir.dt.float32
BF16 = mybir.dt.bfloat16
I32 = mybir.dt.int32
AF = mybir.ActivationFunctionType
ALU = mybir.AluOpType
AX = mybir.AxisListType

D = 1024
DEPTH = 2
CH = 64
HD = 128
MW = 512
NH = 4
NE = 32
DFF = 512
D_IN = 8708
ALPHA = (2 * DEPTH) ** 0.25
LN_EPS = 1e-5
HN_EPS = 1e-6
N_CORES = 8

MOE_STOP = 0
PRECAST = True
ALWAYS_LOAD = False
OOB_IDX = 1048576.0
ENGS = ("pe", "act", "dve", "pool", "sp")
NDQ = 12


class Prog:
    def __init__(self, same_sync=("act", "dve", "pool")):
        self.nc = bass.Bass("TRN2", target_bir_lowering=False)
        self.ops = {e: [] for e in ENGS}
        self.streams = (["pe", "act", "dve", "pool"] + [("dq", i) for i in range(NDQ)] + [("sq", i) for i in range(NDQ)]
                        + [("aq", i) for i in range(NDQ)])
        self.nq = {"sp": 0, "pool": 0, "act": 0}
        self.cnt = {s: 0 for s in self.streams}
        self.clock = {e: {s: 0 for s in self.streams} for e in ENGS}
        self.evclock = {}
        self.res = {}
        self.same_sync = set(same_sync)
        self.ndma = 0
        self._stack = []
        self.nops = 0
        self._bregs = {}

    def raw_sbuf(self, name, shape, dtype=F32):
        cm = self.nc.sbuf_tensor(name, list(shape), dtype)
        t = cm.__enter__()
        self._stack.append(cm)
        return t

    def raw_psum(self, name, shape, dtype=F32):
        cm = self.nc.psum_tensor(name, list(shape), dtype)
        t = cm.__enter__()
        self._stack.append(cm)
        return t

    def dram(self, name, shape, dtype=F32, kind="Internal"):
        return self.nc.dram_tensor(name, list(shape), dtype, kind=kind).ap()

    def _deps(self, eng, reads, writes):
        deps = {}

        def add(s, i):
            if s == eng and eng not in self.same_sync:
                return
            if deps.get(s, 0) < i:
                deps[s] = i

        for r in reads:
            st = self.res.get(r)
            if st and st["w"]:
                add(*st["w"])
        for w in writes:
            st = self.res.get(w)
            if st:
                if st["w"]:
                    add(*st["w"])
                for s, i in st["r"].items():
                    add(s, i)
        waits = []
        ck = self.clock[eng]
        for s, i in deps.items():
            if ck[s] >= i:
                continue
            waits.append((s, i))
            evc = self.evclock.get((s, i))
            if evc:
                for k, v in evc.items():
                    if ck[k] < v:
                        ck[k] = v
            ck[s] = i
        return waits

    def _mark(self, ev, reads, writes):
        for r in reads:
            st = self.res.setdefault(r, {"w": None, "r": {}})
            if st["r"].get(ev[0], 0) < ev[1]:
                st["r"][ev[0]] = ev[1]
        for w in writes:
            self.res[w] = {"w": ev, "r": {}}

    def op(self, eng, fn, reads=(), writes=(), sig=True):
        waits = self._deps(eng, reads, writes)
        idx = self.cnt[eng] + 1
        if sig:
            self.cnt[eng] = idx
            self.evclock[(eng, idx)] = dict(self.clock[eng])
        ev = (eng, idx)
        self._mark(ev, reads, writes)
        self.ops[eng].append((waits, fn, (eng, 1) if sig else None))
        self.nops += 1
        return ev

    def dma(self, out, in_, reads=(), writes=(), q="sp", **kw):
        k = self.nq[q]
        self.nq[q] += 1
        slot = ({"sp": "dq", "pool": "sq", "act": "aq"}[q], k % NDQ)
        idx = self.cnt[slot] + 1
        waits = self._deps(q, reads, writes)
        if idx > 1 and self.clock[q][slot] < idx - 1:
            waits.append((slot, idx - 1))
            self.clock[q][slot] = idx - 1
        self.cnt[slot] = idx
        self.evclock[(slot, idx)] = dict(self.clock[q])
        ev = (slot, idx)
        self._mark(ev, reads, writes)
        self.ops[q].append((waits, lambda e: e.dma_start(out=out, in_=in_, **kw), (slot, 16)))
        self.nops += 1
        return ev

    def idma(self, out, out_off, in_, in_off, reads=(), writes=(), bounds=None):
        k = self.nq["pool"]
        self.nq["pool"] += 1
        slot = ("sq", k % NDQ)
        idx = self.cnt[slot] + 1
        waits = self._deps("pool", reads, writes)
        if idx > 1 and self.clock["pool"][slot] < idx - 1:
            waits.append((slot, idx - 1))
            self.clock["pool"][slot] = idx - 1
        self.cnt[slot] = idx
        self.evclock[(slot, idx)] = dict(self.clock["pool"])
        ev = (slot, idx)
        self._mark(ev, reads, writes)
        if bounds is None:
            self.ops["pool"].append((waits, lambda e: e.indirect_dma_start(out, out_off, in_, in_off), (slot, 16)))
        else:
            self.ops["pool"].append((waits, lambda e: e.indirect_dma_start(out, out_off, in_, in_off, bounds_check=self._breg(e, bounds),
                                                                           oob_is_err=False), (slot, 16)))
        self.nops += 1
        return ev

    def _breg(self, e, val):
        if val not in self._bregs:
            self._bregs[val] = e.to_reg(val)
        return self._bregs[val]

    def barrier(self):
        for eng in ENGS:
            waits = []
            for s in self.streams:
                if self.cnt[s] > self.clock[eng][s]:
                    waits.append((s, self.cnt[s]))
                    self.clock[eng][s] = self.cnt[s]
            self.ops[eng].append((waits, None, None))
        self.res = {}

    def mm(self, out, lhsT, rhs, start, stop, reads, writes, sig=None):
        if sig is None:
            sig = stop
        return self.op("pe", lambda e: e.matmul(out, lhsT, rhs, start=start, stop=stop), reads, writes, sig)

    def tr(self, out, in_, ident, reads, writes, sig=True):
        return self.op("pe", lambda e: e.transpose(out, in_, ident), reads, writes, sig)

    def act(self, out, in_, func, reads, writes, bias=0.0, scale=1.0, accum_out=None):
        if accum_out is None:
            return self.op("act", lambda e: e.activation(out=out, in_=in_, func=func, bias=bias, scale=scale), reads, writes)
        return self.op("act", lambda e: e.activation(out=out, in_=in_, func=func, bias=bias, scale=scale, accum_out=accum_out), reads, writes)

    def tt(self, eng, out, in0, in1, op, reads, writes):
        return self.op(eng, lambda e: e.tensor_tensor(out, in0, in1, op), reads, writes)

    def ts(self, eng, out, in0, s1, s2, op0, op1, reads, writes):
        if s2 is None:
            return self.op(eng, lambda e: e.tensor_scalar(out, in0, s1, None, op0), reads, writes)
        return self.op(eng, lambda e: e.tensor_scalar(out, in0, s1, s2, op0, op1), reads, writes)

    def stt(self, eng, out, in0, scalar, in1, op0, op1, reads, writes):
        return self.op(eng, lambda e: e.scalar_tensor_tensor(out, in0, scalar, in1, op0, op1), reads, writes)

    def cp(self, eng, out, in_, reads, writes):
        if eng == "act":
            return self.op("act", lambda e: e.copy(out, in_), reads, writes)
        return self.op(eng, lambda e: e.tensor_copy(out, in_), reads, writes)

    def emit(self):
        nc = self.nc
        for eng in ENGS:
            pass
        self.barrier()
        sem_cms = [nc.semaphore("s_%s" % (s if isinstance(s, str) else "%s%d" % s)) for s in self.streams]
        sems = {}
        for s, cm in zip(self.streams, sem_cms):
            sems[s] = cm.__enter__()
        mult = {s: (1 if isinstance(s, str) else 16) for s in self.streams}
        blk_cm = nc.Block()
        block = blk_cm.__enter__()

        def run(e_name):
            def body(eng):
                for waits, fn, inc in self.ops[e_name]:
                    for s, i in waits:
                        eng.wait_ge(sems[s], i * mult[s])
                    if fn is None:
                        continue
                    ins = fn(eng)
                    if inc is not None:
                        ins.then_inc(sems[inc[0]], inc[1])
            return body

        block.tensor(run("pe"))
        block.scalar(run("act"))
        block.vector(run("dve"))
        block.gpsimd(run("pool"))
        block.sync(run("sp"))
        blk_cm.__exit__(None, None, None)
        for cm in reversed(sem_cms):
            cm.__exit__(None, None, None)
        for cm in reversed(self._stack):
            cm.__exit__(None, None, None)
        return nc


class Mem:
    def __init__(self, p, kbytes=190):
        self.n32 = kbytes * 256
        self.big = p.raw_sbuf("big", [128, self.n32], F32)
        self.off = 0
        self.base = 0

    def reset(self, keep=None):
        self.off = self.base if keep is None else keep

    def f32(self, n, parts=128):
        a = self.off
        self.off += (n + 7) // 8 * 8
        assert self.off <= self.n32, "SBUF overflow %d" % self.off
        return self.big[0:parts, a:a + n]

    def bf16(self, n, parts=128):
        n32 = (n + 1) // 2
        a = self.off
        self.off += (n32 + 7) // 8 * 8
        assert self.off <= self.n32, "SBUF overflow %d" % self.off
        return self.big[0:parts, a:a + n32].bitcast(BF16)


FM_AQ, FM_AF, FM_AG, FM_BQ, FM_BK, FM_CQ, FM_CK, FM_CG, FM_GA, FM_GB, FM_GC, FM_BF = (
    0, 512, 1024, 1536, 2048, 2560, 3072, 3584, 4096, 5120, 6144, 7168)
FM_ROWS = 7172
WC_AQ, WC_AF, WC_AI, WC_AG, WC_BQ, WC_BK, WC_BV, WC_BF, WC_CQ, WC_CK, WC_CV, WC_CG, WC_GA, WC_GB, WC_GC = (
    0, 512, 1024, 1536, 2048, 2560, 3072, 3584, 3588, 4100, 4612, 5124, 5636, 6660, 7684)


class Ctx:
    pass


def precast_experts(c, lw, e0, e1):
    p = c.p
    wgv_ = c.w_gate[lw].rearrange("e (p j) f -> (e p) (j f)", j=8)
    wuv_ = c.w_up[lw].rearrange("e (p j) f -> (e p) (j f)", j=8)
    wdv_ = c.w_down[lw].rearrange("e (p j) d -> (e p) (j d)", j=4)
    for e_ in range(e0, e1):
        r0 = e_ * 128
        p.dma(c.wgb[lw][r0:r0 + 128, :], wgv_[r0:r0 + 128, :], q="pool", max_dma_last_dim=4096)
        p.dma(c.wub[lw][r0:r0 + 128, :], wuv_[r0:r0 + 128, :], q="pool", max_dma_last_dim=4096)
        p.dma(c.wdb[lw][r0:r0 + 128, :], wdv_[r0:r0 + 128, :], q="pool", max_dma_last_dim=4096)


def phase_inproj(c, l, xin):
    p, mem, T = c.p, c.mem, c.T
    p.barrier()
    mem.reset()
    NT = T // 128
    NG = T // 512
    xT = mem.bf16(8 * T).rearrange("p (k t) -> p k t", k=8)
    xs = [mem.f32(D) for _ in range(2)]
    wb = [mem.bf16(8 * 128).rearrange("p (k c) -> p k c", k=8) for _ in range(4)]
    w512 = [mem.bf16(8 * 512).rearrange("p (k c) -> p k c", k=8) for _ in range(2)]
    stage = [mem.f32(T) for _ in range(2)]
    stm = [mem.f32(512) for _ in range(2)]
    tmp1 = [mem.f32(512) for _ in range(2)]
    tmp2 = [mem.f32(512) for _ in range(2)]
    c.cosT = mem.f32(T)
    c.sinT = mem.f32(T)
    p.dma(c.cosT, c.cd["cosT"], writes=["consts"])
    p.dma(c.sinT, c.cd["sinT"], writes=["consts"])
    if c.precast and l == 0:
        precast_experts(c, 0, 0, NE)
    w_l = c.w_in[l]
    wv = w_l.rearrange("(k p) c -> p k c", p=128)

    jobs = []
    for h in range(4):
        jobs.append((FM_AQ + 128 * h, WC_AQ + 128 * h, 128, "copy"))
        jobs.append((FM_AF + 128 * h, WC_AF + 128 * h, 128, "copy"))
        jobs.append((FM_AG + 128 * h, WC_AG + 128 * h, 128, "sig"))
        jobs.append((FM_BQ + 128 * h, WC_BQ + 128 * h, 128, "qscale"))
        jobs.append((FM_BK + 128 * h, WC_BK + 128 * h, 128, "copy"))
        jobs.append((FM_CQ + 128 * h, WC_CQ + 128 * h, 128, "rope"))
        jobs.append((FM_CK + 128 * h, WC_CK + 128 * h, 128, "rope"))
        jobs.append((FM_CG + 128 * h, WC_CG + 128 * h, 128, "silu"))
    for j in range(8):
        jobs.append((FM_GA + 128 * j, WC_GA + 128 * j, 128, "sig"))
        jobs.append((FM_GB + 128 * j, WC_GB + 128 * j, 128, "sig"))
        jobs.append((FM_GC + 128 * j, WC_GC + 128 * j, 128, "sig"))
    jobs.append((FM_BF, WC_BF, 4, "copy"))

    wcnt = 0
    scnt = 0
    pcnt = 0
    ecnt = 0
    for s in range(c.NSEQ):
        for i in range(NT):
            xb = xs[i % 2]
            p.dma(xb, xin[s * T + i * 128: s * T + (i + 1) * 128, :], writes=[("xs", i % 2)])
            for half in range(2):
                bank = c.ps[pcnt % 4]
                bk = ("ps", pcnt % 4)
                pcnt += 1
                for j in range(4):
                    kc = half * 4 + j
                    p.tr(bank[:, j * 128:(j + 1) * 128], xb[:, kc * 128:(kc + 1) * 128], c.ident,
                         reads=[("xs", i % 2), "ident"], writes=[bk], sig=(j == 3))
                eng = "act" if ecnt % 2 == 0 else "dve"
                ecnt += 1
                p.cp(eng, xT[:, half * 4:(half + 1) * 4, i * 128:(i + 1) * 128],
                     bank[:, :].rearrange("p (j c) -> p j c", j=4), reads=[bk], writes=[("xT", i)])
        for (row, col, nc_, kind) in jobs:
            nw = 2 if kind == "rope" else 1
            wids = []
            for v in range(nw):
                wi = wcnt % 4
                wcnt += 1
                wids.append(wi)
                if v == 0:
                    p.dma(wb[wi][:, :, 0:nc_], wv[:, :, col:col + nc_], writes=[("wb", wi)], q="pool")
                else:
                    p.dma(wb[wi][:, :, 0:64], wv[:, :, col + 64:col + 128], writes=[("wb", wi)], q="pool")
                    p.dma(wb[wi][:, :, 64:128], wv[:, :, col:col + 64], writes=[("wb", wi)], q="pool")
            sb = scnt % 2
            scnt += 1
            st = stage[sb]
            for g in range(NG):
                xr = [("xT", 4 * g + j) for j in range(4)]
                banks = []
                for v in range(nw):
                    bi = pcnt % 4
                    pcnt += 1
                    banks.append(bi)
                    for kc in range(8):
                        p.mm(c.ps[bi][0:nc_, :], wb[wids[v]][:, kc, 0:nc_], xT[:, kc, g * 512:(g + 1) * 512],
                             start=(kc == 0), stop=(kc == 7), reads=xr + [("wb", wids[v])], writes=[("ps", bi)])
                dst = st[0:nc_, g * 512:(g + 1) * 512]
                src = c.ps[banks[0]][0:nc_, :]
                rd = [("ps", banks[0])]
                wr = [("stage", sb, g)]
                if kind == "copy":
                    eng = "act" if ecnt % 2 == 0 else "dve"
                    ecnt += 1
                    p.cp(eng, dst, src, rd, wr)
                elif kind == "sig":
                    p.act(dst, src, AF.Sigmoid, rd, wr)
                elif kind == "silu":
                    p.act(dst, src, AF.Silu, rd, wr)
                elif kind == "qscale":
                    p.act(dst, src, AF.Copy, rd, wr, scale=float(HD ** -0.5))
                elif kind == "rope":
                    tb = ecnt % 2
                    ecnt += 1
                    tok = slice(g * 512, (g + 1) * 512)
                    p.tt("dve", tmp1[tb], src, c.cosT[:, tok], ALU.mult, rd + ["consts"], [("tmp1", tb)])
                    p.tt("dve", tmp2[tb], c.ps[banks[1]][:, :], c.sinT[:, tok], ALU.mult,
                         [("ps", banks[1]), "consts"], [("tmp2", tb)])
                    p.tt("pool", dst, tmp1[tb], tmp2[tb], ALU.add, [("tmp1", tb), ("tmp2", tb)], wr)
            p.dma(c.FM[l % 1][row:row + nc_, s * T:(s + 1) * T], st[0:nc_, :],
                  reads=[("stage", sb, g) for g in range(NG)])
        for vi, col in enumerate((WC_AI, WC_BV, WC_CV)):
            wi = vi % 2
            p.dma(w512[wi], wv[:, :, col:col + 512], writes=[("w512", wi)], q="pool")
            for i in range(NT):
                bi = pcnt % 4
                pcnt += 1
                for kc in range(8):
                    p.mm(c.ps[bi][:, :], xT[:, kc, i * 128:(i + 1) * 128], w512[wi][:, kc, :],
                         start=(kc == 0), stop=(kc == 7), reads=[("xT", i), ("w512", wi)], writes=[("ps", bi)])
                sb = scnt % 2
                scnt += 1
                eng = "act" if ecnt % 2 == 0 else "dve"
                ecnt += 1
                p.cp(eng, stm[sb], c.ps[bi][:, :], [("ps", bi)], [("stm", sb)])
                p.dma(c.TM[s * T + i * 128: s * T + (i + 1) * 128, vi * 512:(vi + 1) * 512], stm[sb],
                      reads=[("stm", sb)])


def phase_rec(c, l, br):
    p, mem, T = c.p, c.mem, c.T
    p.barrier()
    mem.reset()
    SP = min(T, 1024)
    NSP = T // SP
    NCH = SP // 64
    isA = br == "A"
    if c.precast and l + 1 < DEPTH and not c.dbg:
        precast_experts(c, l + 1, 0 if isA else 2 * NE // 3, NE // 3 if isA else NE)
    qrow, krow, grow = (FM_AQ, FM_AF, FM_AG) if isA else (FM_CQ, FM_CK, FM_CG)
    vcol = 0 if isA else 1024
    yrow = 0 if isA else 1024
    FM = c.FM[0]
    qe = [[mem.bf16(SP) for _ in range(4)] for _ in range(2)]
    ke = [[mem.bf16(SP) for _ in range(4)] for _ in range(2)]
    kh = [[mem.bf16(SP) for _ in range(4)] for _ in range(2)]
    V = [mem.bf16(NCH * 512, parts=64).rearrange("p (c f) -> p c f", c=NCH) for _ in range(2)]
    elast = [mem.f32(4 * NCH).rearrange("p (h c) -> p h c", h=4) for _ in range(2)]
    Tm = [[mem.f32(SP) for _ in range(6)] for _ in range(2)]
    S = mem.f32(512)
    Sb = [mem.bf16(512) for _ in range(2)]
    scnt = [0]
    sm = [mem.bf16(256, parts=64) for _ in range(2)]
    kt = [mem.bf16(512, parts=64) for _ in range(2)]
    tri = mem.f32(64, parts=64)
    rmask = mem.f32(SP)
    ones = mem.bf16(128)
    osb = [mem.f32(512) for _ in range(4)]
    gt = [mem.f32(512) for _ in range(2)]
    w1 = [mem.f32(512) for _ in range(2)]
    w2 = [mem.f32(512) for _ in range(2)]
    wbf = [mem.bf16(512) for _ in range(2)]
    yb = [mem.bf16(512) for _ in range(2)]
    lbt = mem.f32(4)
    omt = mem.f32(4)
    rdec = mem.f32(3 * 4 * 64).rearrange("p (a h i) -> p a h i", a=3, h=4)
    rel = mem.f32(4)
    psO = [c.ps[h] for h in range(4)]
    psS32, psT32, psN, psM = c.ps[4], c.ps[5], c.ps[6], c.ps[7]
    psS = [psS32[0:64, 0:256], psS32[0:64, 256:512]]
    psTb = psT32[:, :].bitcast(BF16)
    psT = [psTb[0:64, 0:512], psTb[0:64, 512:1024]]

    p.dma(tri, c.cd["tri64"], writes=["tri"])
    p.dma(rmask, c.cd["rmask"][:, 0:SP], writes=["rmask"])
    p.dma(ones, c.cd["ones128"], writes=["ones"], q="pool")
    if isA:
        p.dma(lbt, c.lb_d[l], writes=["lbt"])
        p.ts("dve", omt, lbt, -1.0, 1.0, ALU.mult, ALU.add, ["lbt"], ["omt"])
    else:
        p.dma(rdec.rearrange("p a h i -> p (a h i)"), c.cd["rdec"], writes=["rdec"])
        p.dma(rel, c.cd["rel"], writes=["rel"])
    spans = [(s, sp) for s in range(c.NSEQ) for sp in range(NSP)]
    v3 = lambda a: a.rearrange("p (c i) -> p c i", i=64)

    def prep_gen(n):
        s, sp = spans[n]
        bs = n % 2
        t0 = s * T + sp * SP
        tok = slice(t0, t0 + SP)
        p.dma(V[bs], c.TM[t0:t0 + SP, vcol:vcol + 512].rearrange("(c p) f -> p c f", p=64), writes=[("V", bs)], q="pool")
        yield
        for h in range(4):
            hb = h % 2
            T0, T1, T2, T3, T4, T5 = Tm[hb]
            k0, k1, k2, k3, k4, k5 = [("Tm", hb, j) for j in range(6)]
            qk, kk_, hk, ek = ("qe", bs, h), ("ke", bs, h), ("kh", bs, h), ("elast", bs)
            if isA:
                p.dma(T0, FM[krow + 128 * h: krow + 128 * h + 128, tok], writes=[k0])
                p.dma(T3, FM[qrow + 128 * h: qrow + 128 * h + 128, tok], writes=[k3])
                p.act(T0, T0, AF.Sigmoid, [k0], [k0])
                yield
                p.act(T0, T0, AF.Identity, [k0, "omt", "lbt"], [k0], bias=lbt[:, h:h + 1], scale=omt[:, h:h + 1])
                yield
                p.act(T1, T0, AF.Ln, [k0], [k1])
                yield
                p.op("dve", lambda e, T2=T2, T1=T1: e.tensor_tensor_scan(T2, rmask, T1, 0.0, ALU.mult, ALU.add),
                     [k1, "rmask"], [k2])
                yield
                p.act(T0, T0, AF.Identity, [k0], [k0], bias=c.oneb, scale=-1.0)
                yield
                p.act(T1, T2, AF.Exp, [k2], [k1])
                yield
                p.tt("dve", qe[bs][h], T3, T1, ALU.mult, [k3, k1], [qk])
                yield
                p.act(T4, T2, AF.Exp, [k2], [k4], scale=-1.0)
                yield
                p.tt("pool", ke[bs][h], T0, T4, ALU.mult, [k0, k4], [kk_])
                yield
                p.act(elast[bs][:, h, :], v3(T2)[:, :, 63], AF.Exp, [k2], [ek])
                p.tt("dve", v3(T5), v3(T2)[:, :, 63:64].to_broadcast([128, NCH, 64]), v3(T2), ALU.subtract, [k2], [k5])
                yield
                p.act(T5, T5, AF.Exp, [k5], [k5])
                yield
                p.tt("pool", kh[bs][h], T0, T5, ALU.mult, [k0, k5], [hk])
                yield
            else:
                bc = lambda a: a.unsqueeze(1).to_broadcast([128, NCH, 64])
                p.dma(T3, FM[qrow + 128 * h: qrow + 128 * h + 128, tok], writes=[k3])
                p.dma(T0, FM[krow + 128 * h: krow + 128 * h + 128, tok], writes=[k0])
                p.tt("dve", v3(qe[bs][h]), v3(T3), bc(rdec[:, 0, h, :]), ALU.mult, [k3, "rdec"], [qk])
                yield
                p.tt("dve", v3(ke[bs][h]), v3(T0), bc(rdec[:, 1, h, :]), ALU.mult, [k0, "rdec"], [kk_])
                yield
                p.tt("pool", v3(kh[bs][h]), v3(T0), bc(rdec[:, 2, h, :]), ALU.mult, [k0, "rdec"], [hk])
                p.cp("dve", elast[bs][:, h, :], rel[:, h:h + 1].to_broadcast([128, NCH]), ["rel"], [ek])
                yield

    def post_gen(gtok, g):
        for h in range(4):
            b = (g * 4 + h) % 2
            p.dma(gt[b], FM[grow + 128 * h: grow + 128 * h + 128, gtok], writes=[("gt", b)])
            oc = osb[h]
            if not isA:
                p.cp("pool", wbf[b], osb[h], [("osb", h)], [("wbf", b)])
                p.mm(psM[:, :], ones, wbf[b], True, True, ["ones", ("wbf", b)], ["psM"])
                yield
                p.tt("dve", osb[h], osb[h], psM[:, :], ALU.subtract, [("osb", h), "psM"], [("osb", h)])
                yield
            p.act(wbf[b], oc, AF.Square, [("osb", h)], [("wbf", b)])
            p.mm(psM[:, :], ones, wbf[b], True, True, ["ones", ("wbf", b)], ["psM"])
            yield
            p.act(w1[b], psM[:, :], AF.Ln, ["psM", "epsb"], [("w1", b)], bias=c.epsb)
            yield
            p.act(w1[b], w1[b], AF.Exp, [("w1", b)], [("w1", b)], scale=-0.5)
            yield
            p.tt("dve", w2[b], oc, w1[b], ALU.mult, [("osb", h), ("w1", b)], [("w2", b)])
            yield
            p.tt("pool", yb[b], w2[b], gt[b], ALU.mult, [("w2", b), ("gt", b)], [("yb", b)])
            p.dma(c.YT[yrow + 128 * h: yrow + 128 * h + 128, gtok], yb[b], reads=[("yb", b)], q="pool")
            yield

    posts = []

    def drain(gens, k):
        for _ in range(k):
            while gens:
                try:
                    next(gens[0])
                    break
                except StopIteration:
                    gens.pop(0)

    def exhaust(gens):
        while gens:
            for _ in gens[0]:
                pass
            gens.pop(0)

    preps = [prep_gen(0)]
    exhaust(preps)
    gcount = 0
    for n, (s, sp) in enumerate(spans):
        bs = n % 2
        t0 = s * T + sp * SP
        if sp == 0:
            p.op("pool", lambda e: e.memset(S, 0.0), ["S"], ["S"])
            p.op("pool", lambda e, o=Sb[scnt[0] % 2]: e.memset(o, 0.0), [], [("Sb", scnt[0] % 2)])
        preps = [prep_gen(n + 1)] if n + 1 < len(spans) else []
        Q, KE, KH, VV, EL = qe[bs], ke[bs], kh[bs], V[bs], elast[bs]

        def stA1(ch):
            cs = slice(ch * 64, ch * 64 + 64)
            b2 = ch % 2
            for h in range(4):
                p.mm(psS[b2][:, h * 64:(h + 1) * 64], KE[h][:, cs], Q[h][:, cs], True, True,
                     [("ke", bs, h), ("qe", bs, h)], [("psS", b2)], sig=(h == 3))
            p.tt("dve", sm[b2].rearrange("p (h t) -> p h t", h=4), psS[b2].rearrange("p (h t) -> p h t", h=4),
                 tri.unsqueeze(1).to_broadcast([64, 4, 64]), ALU.mult, [("psS", b2), "tri"], [("sm", b2)])
            for h in range(4):
                p.tr(psT[b2][:, h * 128:(h + 1) * 128], KH[h][:, cs], c.identb, [("kh", bs, h), "identb"], [("psT", b2)], sig=(h == 3))
            p.cp("act", kt[b2], psT[b2], [("psT", b2)], [("kt", b2)])

        def stA2(ch):
            b2 = ch % 2
            gs = slice((ch % 8) * 64, (ch % 8) * 64 + 64)
            for h in range(4):
                hs = slice(128 * h, 128 * h + 128)
                p.mm(psN[:, hs], kt[b2][:, hs], VV[:, ch, hs], True, True, [("kt", b2), ("V", bs)], ["psN"], sig=(h == 3))
            for h in range(4):
                hs = slice(128 * h, 128 * h + 128)
                p.mm(psO[h][:, gs], VV[:, ch, hs], sm[b2][:, h * 64:(h + 1) * 64], True, False,
                     [("V", bs), ("sm", b2)], [("psO", h)], sig=False)

        def stB(ch):
            nonlocal gcount
            cs = slice(ch * 64, ch * 64 + 64)
            g = ch // 8
            gs = slice((ch % 8) * 64, (ch % 8) * 64 + 64)
            sbi = scnt[0] % 2
            scnt[0] += 1
            for h in range(4):
                hs = slice(128 * h, 128 * h + 128)
                p.mm(psO[h][:, gs], Sb[sbi][:, hs], Q[h][:, cs], False, True, [("Sb", sbi), ("qe", bs, h)], [("psO", h)], sig=True)
            S3 = S.rearrange("p (h v) -> p h v", h=4)
            p.tt("dve", S3, S3, EL[:, :, ch:ch + 1].to_broadcast([128, 4, 128]), ALU.mult, ["S", ("elast", bs)], ["S"])
            p.tt("dve", S, S, psN[:, :], ALU.add, ["S", "psN"], ["S"])
            p.cp("act", Sb[1 - sbi], S, ["S"], [("Sb", 1 - sbi)])
            if ch % 8 == 7:
                exhaust(posts)
                for h in range(4):
                    p.cp("act" if h % 2 else "dve", osb[h], psO[h][:, :], [("psO", h)], [("osb", h)])
                posts.append(post_gen(slice(t0 + g * 512, t0 + g * 512 + 512), gcount))
                gcount += 1

        stA1(0)
        stA2(0)
        for ch in range(NCH):
            if ch + 1 < NCH:
                stA1(ch + 1)
            stB(ch)
            if ch + 1 < NCH:
                stA2(ch + 1)
            drain(posts, 4)
            drain(preps, 5)
        exhaust(preps)
    exhaust(posts)


def phase_fox(c, l):
    p, mem, T = c.p, c.mem, c.T
    p.barrier()
    mem.reset()
    NT = T // 128
    NG = T // 512
    FM = c.FM[0]
    if c.precast and l + 1 < DEPTH and not c.dbg:
        precast_experts(c, l + 1, NE // 3, 2 * NE // 3)
    sd = float(HD ** 0.5)
    qT = mem.bf16(T)
    kT = mem.bf16(T)
    V = mem.bf16(NT * 128).rearrange("p (j v) -> p j v", j=NT)
    B = mem.f32(T)
    cum1 = mem.f32(T, parts=4)
    cum2 = mem.f32(T, parts=4)
    ones4 = mem.f32(T, parts=4)
    negc = mem.f32(NT * 4).rearrange("p (j h) -> p j h", h=4)
    sel = mem.f32(4 * 128, parts=4).rearrange("p (h m) -> p h m", h=4)
    fb = mem.f32(1, parts=4)
    tri = mem.f32(128)
    onesb = mem.bf16(128)
    sq = [mem.bf16(512) for _ in range(2)]
    tmp = [mem.f32(512) for _ in range(4)]
    PT = [mem.bf16(512) for _ in range(5)]
    kmaxs = mem.f32(NG)
    kterm = mem.f32(1)
    rl = [mem.f32(512) for _ in range(2)]
    yb = [mem.bf16(512) for _ in range(2)]
    psA = [c.ps[0], c.ps[1], c.ps[2], c.ps[7]]
    psO = [c.ps[3], c.ps[4]]
    psL = [c.ps[5], c.ps[6]]
    psX = [c.ps[7], c.ps[7]]

    p.dma(tri, c.cd["ntri128"], writes=["tri"])
    p.dma(onesb, c.cd["ones1"], writes=["onesb"], q="pool")
    p.dma(sel.rearrange("p h m -> p (h m)"), c.cd["sel4"], writes=["sel"])
    p.dma(fb, c.fox_bias[l].rearrange("(h o) -> h o", o=1), writes=["fb"])
    p.op("pool", lambda e: e.memset(ones4, 1.0), [], ["ones4"])
    acnt = 0
    xcnt = 0
    gcnt = 0
    pcnt = 0
    for s in range(c.NSEQ):
        tok = slice(s * T, (s + 1) * T)
        p.dma(cum1, FM[FM_BF:FM_BF + 4, tok], writes=["cum1"])
        p.act(cum1, cum1, AF.Sigmoid, ["cum1", "fb"], ["cum1"], bias=fb)
        p.act(cum1, cum1, AF.Ln, ["cum1"], ["cum1"])
        p.op("dve", lambda e: e.tensor_tensor_scan(cum2, ones4, cum1, 0.0, ALU.mult, ALU.add), ["cum1", "ones4"], ["cum2"])
        xb = 0
        xcnt += 1
        for j in range(NT):
            p.tr(psX[xb][:, j * 4:(j + 1) * 4], cum2[:, j * 128:(j + 1) * 128], c.ident[0:4, 0:4],
                 ["cum2", "ident"], [("psA", 3)], sig=(j == NT - 1))
        p.act(negc.rearrange("p j h -> p (j h)"), psX[xb][:, 0:NT * 4], AF.Copy, [("psA", 3)], ["negc"], scale=-1.0)
        for h in range(4):
            hr = slice(128 * h, 128 * h + 128)
            p.dma(qT, FM[FM_BQ + 128 * h: FM_BQ + 128 * h + 128, tok], writes=["qT"], q="pool")
            p.dma(kT, FM[FM_BK + 128 * h: FM_BK + 128 * h + 128, tok], writes=["kT"], q="pool")
            p.dma(V, c.TM[tok, 512 + 128 * h: 512 + 128 * h + 128].rearrange("(j p) v -> p j v", p=128), writes=["V"], q="pool")
            for g in range(NG):
                gs = slice(g * 512, (g + 1) * 512)
                b = acnt % 2
                acnt += 1
                p.act(sq[b], kT[:, gs], AF.Square, ["kT"], [("sq", b)])
                p.mm(psA[b][:, :], onesb, sq[b], True, True, ["onesb", ("sq", b)], [("psA", b)])
                p.op("dve", lambda e, o=kmaxs[:, g:g + 1], i=psA[b][:, :]: e.reduce_max(o, i, AX.X), [("psA", b)], ["kmaxs"])
            p.op("dve", lambda e: e.reduce_max(kterm, kmaxs, AX.X), ["kmaxs"], ["kterm"])
            p.ts("dve", kterm, kterm, -0.5 / sd, None, ALU.mult, None, ["kterm"], ["kterm"])
            for g in range(NG):
                gs = slice(g * 512, (g + 1) * 512)
                b = acnt % 2
                acnt += 1
                xb = 0
                xcnt += 1
                p.act(sq[b], qT[:, gs], AF.Square, ["qT"], [("sq", b)])
                p.mm(psA[b][:, :], onesb, sq[b], True, True, ["onesb", ("sq", b)], [("psA", b)])
                p.act(tmp[b], psA[b][:, :], AF.Copy, [("psA", b)], [("tmp", b)], scale=-0.5 * sd)
                p.mm(psX[xb][:, :], sel[:, h, :], cum2[:, gs], True, True, ["sel", "cum2"], [("psA", 3)])
                p.stt("dve", B[:, gs], psX[xb][:, :], kterm, tmp[b], ALU.add, ALU.add,
                      [("psA", 3), "kterm", ("tmp", b)], [("B", g)])
            blocks = []
            for g in range(NG):
                nk = 4 * g + 4
                for j in range(nk):
                    blocks.append((g, j, nk))
            obs = {}
            for g in range(NG):
                obs[g] = gcnt % 2
                gcnt += 1
            st1 = {}

            def stage1(n):
                nonlocal acnt, pcnt
                g, j, nk = blocks[n]
                jj = j - 4 * g
                t0 = max(0, jj) * 128
                qs = slice(g * 512 + t0, (g + 1) * 512)
                ls = slice(t0, 512)
                b = acnt % 4
                acnt += 1
                pb = pcnt % 5
                pcnt += 1
                p.mm(psA[b][:, ls], kT[:, j * 128:(j + 1) * 128], qT[:, qs], True, True, ["kT", "qT"], [("psA", b)])
                p.tt("dve", tmp[b][:, ls], psA[b][:, ls], B[:, qs], ALU.add, [("psA", b), ("B", g)], [("tmp", b)])
                if jj >= 0:
                    p.tt("pool", tmp[b][:, t0:t0 + 128], tmp[b][:, t0:t0 + 128], tri, ALU.add, [("tmp", b), "tri"], [("tmp", b)])
                p.act(PT[pb][:, ls], tmp[b][:, ls], AF.Exp, [("tmp", b), "negc"], [("PT", pb)], bias=negc[:, j, h:h + 1])
                st1[n] = (pb, ls)

            def stage2(n):
                g, j, nk = blocks[n]
                pb, ls = st1.pop(n)
                ob = obs[g]
                p.mm(psO[ob][:, ls], V[:, j, :], PT[pb][:, ls], j == 0, j == nk - 1, ["V", ("PT", pb)], [("psO", ob)])
                p.mm(psL[ob][:, ls], onesb, PT[pb][:, ls], j == 0, j == nk - 1, ["onesb", ("PT", pb)], [("psL", ob)])
                if j == nk - 1:
                    p.act(rl[ob], psL[ob][:, :], AF.Ln, [("psL", ob)], [("rl", ob)])
                    p.act(rl[ob], rl[ob], AF.Exp, [("rl", ob)], [("rl", ob)], scale=-1.0)
                    p.tt("dve", yb[ob], psO[ob][:, :], rl[ob], ALU.mult, [("psO", ob), ("rl", ob)], [("yb", ob)])
                    p.dma(c.YT[512 + 128 * h: 512 + 128 * h + 128, s * T + g * 512: s * T + (g + 1) * 512], yb[ob],
                          reads=[("yb", ob)])

            LA = 3
            for n in range(min(LA, len(blocks))):
                stage1(n)
            for n in range(len(blocks)):
                if n + LA < len(blocks):
                    stage1(n + LA)
                stage2(n)


def layer_norm_tile(c, hh, outt, G, Bt, tagb, eps_ap):
    p = c.p
    st, mv = c.ln_st[tagb], c.ln_mv[tagb]
    hk, ok = ("hh", tagb), ("lnout", tagb)
    p.op("dve", lambda e: e.bn_stats(st[:, 0:6], hh[:, 0:512]), [hk], [("lnst", tagb)])
    p.op("dve", lambda e: e.bn_stats(st[:, 6:12], hh[:, 512:1024]), [hk], [("lnst", tagb)])
    p.op("dve", lambda e: e.bn_aggr(mv[:, 0:2], st[:, 0:12]), [("lnst", tagb)], [("lnmv", tagb)])
    p.act(mv[:, 2:3], mv[:, 1:2], AF.Sqrt, [("lnmv", tagb), "lneps"], [("lnmv", tagb)], bias=eps_ap)
    p.op("dve", lambda e: e.reciprocal(mv[:, 2:3], mv[:, 2:3]), [("lnmv", tagb)], [("lnmv", tagb)])
    p.ts("dve", mv[:, 3:4], mv[:, 0:1], -1.0, None, ALU.mult, None, [("lnmv", tagb)], [("lnmv", tagb)])
    p.act(outt, hh, AF.Identity, [hk, ("lnmv", tagb)], [ok], bias=mv[:, 3:4])
    p.stt("dve", outt, outt, mv[:, 2:3], G, ALU.mult, ALU.mult, [ok, ("lnmv", tagb), "lnG"], [ok])
    p.tt("pool", outt, outt, Bt, ALU.add, [ok, "lnG"], [ok])


def phase_merge(c, l, xin, xout):
    p, mem, T = c.p, c.mem, c.T
    p.barrier()
    mem.reset()
    NTOK = c.NSEQ * T
    FM = c.FM[0]
    wbr = mem.bf16(3 * 4 * 1024).rearrange("p (b k c) -> p b k c", b=3, k=4)
    wo = mem.bf16(8 * 1024).rearrange("p (k c) -> p k c", k=8)
    G = mem.f32(1024)
    Bt = mem.f32(1024)
    eps = mem.f32(1)
    yT = [mem.bf16(3 * 4 * 512).rearrange("p (b k t) -> p b k t", b=3, k=4) for _ in range(2)]
    gts = [mem.f32(512) for _ in range(6)]
    tt_ = [mem.f32(512) for _ in range(6)]
    mT = [mem.bf16(8 * 512).rearrange("p (k t) -> p k t", k=8) for _ in range(2)]
    xt = [mem.f32(1024) for _ in range(2)]
    hh = [mem.f32(1024) for _ in range(2)]
    xo = [mem.f32(1024) for _ in range(2)]
    c.ln_st = [mem.f32(12) for _ in range(2)]
    c.ln_mv = [mem.f32(4) for _ in range(2)]
    p.dma(wbr.rearrange("p b k c -> p (b k) c"), c.w_branch[l].rearrange("b (k p) c -> p (b k) c", p=128), writes=["wbr"], q="pool")
    p.dma(wo, c.w_out[l].rearrange("(k p) c -> p k c", p=128), writes=["wo"], q="pool")
    p.dma(G, c.ln1_g[l].partition_broadcast(128), writes=["lnG"])
    p.dma(Bt, c.ln1_b[l].partition_broadcast(128), writes=["lnG"])
    p.op("pool", lambda e: e.memset(eps, LN_EPS), [], ["lneps"])
    cnts = {"g": 0, "pc": 0, "t": 0}
    NGR = NTOK // 512

    def branch_units(g):
        gtok = slice(g * 512, (g + 1) * 512)
        yb = g % 2
        p.dma(yT[yb].rearrange("p b k t -> p (b k) t"), c.YT[:, gtok].rearrange("(bk p) t -> p bk t", p=128), writes=[("yT", yb)])
        for cc in range(8):
            ts_ = []
            for br in range(3):
                bi = cnts["pc"] % 6
                cnts["pc"] += 1
                gi = cnts["g"] % 6
                cnts["g"] += 1
                for kc in range(4):
                    p.mm(c.ps[bi][:, :], wbr[:, br, kc, cc * 128:(cc + 1) * 128], yT[yb][:, br, kc, :], kc == 0, kc == 3,
                         ["wbr", ("yT", yb)], [("ps", bi)])
                row = FM_GA + br * 1024 + cc * 128
                p.dma(gts[gi], FM[row:row + 128, gtok], writes=[("gts", gi)])
                p.tt("dve", tt_[gi], c.ps[bi][:, :], gts[gi], ALU.mult, [("ps", bi), ("gts", gi)], [("tt", gi)])
                ts_.append(gi)
            a_, b_, d_ = ts_
            p.tt("pool", tt_[a_], tt_[a_], tt_[b_], ALU.add, [("tt", a_), ("tt", b_)], [("tt", a_)])
            p.tt("dve", mT[yb][:, cc, :], tt_[a_], tt_[d_], ALU.add, [("tt", a_), ("tt", d_)], [("mT", yb, cc)])
            yield

    def out_units(g):
        yb = g % 2
        for i in range(4):
            tb = cnts["t"] % 2
            cnts["t"] += 1
            rows = slice(g * 512 + i * 128, g * 512 + (i + 1) * 128)
            p.dma(xt[tb], xin[rows, :], writes=[("xt", tb)])
            for half in range(2):
                for cc in range(8):
                    p.mm(c.ps[6 + half][:, :], mT[yb][:, cc, i * 128:(i + 1) * 128], wo[:, cc, half * 512:(half + 1) * 512],
                         cc == 0, cc == 7, [("mT", yb, cc), "wo"], [("ps", 6 + half)])
                p.stt("dve", hh[tb][:, half * 512:(half + 1) * 512], xt[tb][:, half * 512:(half + 1) * 512], float(ALPHA),
                      c.ps[6 + half][:, :], ALU.mult, ALU.add, [("xt", tb), ("ps", 6 + half)], [("hh", tb)])
            layer_norm_tile(c, hh[tb], xo[tb], G, Bt, tb, eps)
            p.dma(xout[rows, :], xo[tb], reads=[("lnout", tb)], q="pool")
            yield

    for _ in branch_units(0):
        pass
    for g in range(NGR):
        bu = branch_units(g + 1) if g + 1 < NGR else iter(())
        ou = out_units(g)
        for k in range(4):
            next(bu, None)
            next(bu, None)
            next(ou, None)
        for _ in bu:
            pass


def phase_moe(c, l, xin, xout):
    p, mem, T = c.p, c.mem, c.T
    p.barrier()
    mem.reset()
    N = c.NSEQ * T
    NT = N // 128
    NB = 2 * NT + NE
    NR = NB * 128
    IOA = bass.IndirectOffsetOnAxis
    E1 = mem.f32(NT * 32).rearrange("p (i e) -> p i e", e=32)
    E2 = mem.f32(NT * 32).rearrange("p (i e) -> p i e", e=32)
    RK = mem.f32(NT * 32).rearrange("p (i e) -> p i e", e=32)
    info = mem.f32(NT * 4).rearrange("p (i k a) -> p i k a", k=2, a=2)
    dest = [mem.f32(NT).bitcast(I32) for _ in range(2)]
    destf = mem.f32(NT)
    widx = mem.f32(NB).bitcast(I32)
    carry = mem.f32(32)
    wr = mem.f32(8 * 36).rearrange("p (k c) -> p k c", k=8)
    Ls = mem.bf16(128)
    onesb = mem.bf16(128)
    G = mem.f32(1024)
    Bt = mem.f32(1024)
    eps = mem.f32(1)
    pcol = mem.f32(1)
    keep = mem.off
    xt = [mem.f32(1024) for _ in range(2)]
    x1T = [mem.f32(8 * 128).rearrange("p (k t) -> p k t", k=8) for _ in range(2)]
    lg = [mem.f32(4 * 36) for _ in range(2)]
    smb = [mem.f32(352) for _ in range(2)]
    Mb = [mem.bf16(4 * 32) for _ in range(2)]
    p.dma(wr[:, :, 0:4], c.w_rg[l].rearrange("(k p) c -> p k c", p=128), writes=["wr"], allow_slow_non_contiguous=True)
    p.dma(wr[:, :, 4:36], c.w_re[l].rearrange("(k p) c -> p k c", p=128), writes=["wr"], allow_slow_non_contiguous=True)
    p.dma(Ls, c.cd["lstrict"], writes=["Ls"], q="pool")
    p.dma(onesb, c.cd["ones1"], writes=["onesb"], q="pool")
    p.dma(G, c.ln2_g[l].partition_broadcast(128), writes=["lnG"])
    p.dma(Bt, c.ln2_b[l].partition_broadcast(128), writes=["lnG"])
    p.dma(pcol, c.cd["pcol"], writes=["pcol"])
    p.op("pool", lambda e: e.memset(eps, LN_EPS), [], ["lneps"])
    p.op("pool", lambda e: e.memset(carry, 0.0), [], ["carry"])
    p.dma(info.bitcast(I32)[:, :, 0, 0], c.cd["tokid"][:, 0:NT], writes=["info"], allow_slow_non_contiguous=True)
    p.dma(info.bitcast(I32)[:, :, 1, 0], c.cd["tokid"][:, 0:NT], writes=["info"], allow_slow_non_contiguous=True)
    psX = [c.ps[0], c.ps[1]]
    psLg, psR = [c.ps[2], c.ps[3]], [c.ps[4], c.ps[5]]
    TB = 4
    for bt in range(NT // TB):
        b = bt % 2
        i0 = bt * TB
        S_ = smb[b]
        sk = ("sm", b)
        off = [0]

        def carve(n):
            a_ = S_[:, off[0]:off[0] + n]
            off[0] += n
            return a_
        gmax, gsum, gp, v1, v2, dd, p1 = [carve(4) for _ in range(7)]
        og = carve(16).rearrange("p (t g) -> p t g", t=4)
        ge = carve(16).rearrange("p (t g) -> p t g", t=4)
        ing = carve(32).rearrange("p (t e) -> p t e", t=4)
        ing2 = carve(32).rearrange("p (t e) -> p t e", t=4)
        oh1 = carve(32).rearrange("p (t e) -> p t e", t=4)
        oh2 = carve(32).rearrange("p (t e) -> p t e", t=4)
        tmp4 = carve(128).rearrange("p (t g e) -> p t g e", t=4, g=4)
        for t in range(TB):
            i = i0 + t
            xb = i % 2
            p.dma(xt[xb], xin[i * 128:(i + 1) * 128, :], writes=[("xt", xb)])
            for half in range(2):
                for j in range(4):
                    kc = half * 4 + j
                    p.tr(psX[half][:, j * 128:(j + 1) * 128], xt[xb][:, kc * 128:(kc + 1) * 128], c.ident, [("xt", xb), "ident"],
                         [("psX", half)], sig=(j == 3))
                p.cp("act" if half else "dve", x1T[xb][:, half * 4:(half + 1) * 4, :], psX[half][:, :].rearrange("p (j t) -> p j t", j=4),
                     [("psX", half)], [("x1T", xb)])
            for kc in range(8):
                p.mm(psLg[b][:, t * 36:(t + 1) * 36], x1T[xb][:, kc, :], wr[:, kc, :], kc == 0, kc == 7, [("x1T", xb), "wr"], [("psLg", b)])
        p.cp("act", lg[b], psLg[b][:, 0:TB * 36], [("psLg", b)], [("lg", b)])
        L3 = lg[b].rearrange("p (t c) -> p t c", t=TB)
        Lg = L3[:, :, 0:4]
        Le = L3[:, :, 4:36].rearrange("p t (g e) -> p t g e", g=4)
        lk = ("lg", b)
        bc3 = lambda a_, n: a_.unsqueeze(2).to_broadcast([128, TB, n])
        p.op("dve", lambda e, o=gmax, i_=Lg: e.reduce_max(o, i_, AX.X), [lk], [sk])
        p.tt("dve", og, Lg, bc3(gmax, 4), ALU.is_equal, [lk, sk], [sk])
        p.tt("dve", ge, Lg, bc3(gmax, 4), ALU.subtract, [lk, sk], [sk])
        p.act(ge, ge, AF.Exp, [sk], [sk])
        p.op("dve", lambda e, o=gsum, i_=ge: e.reduce_sum(o, i_, AX.X), [sk], [sk])
        p.op("dve", lambda e, o=gp, i_=gsum: e.reciprocal(o, i_), [sk], [sk])
        p.tt("dve", tmp4, Le, og.unsqueeze(3).to_broadcast([128, TB, 4, 8]), ALU.mult, [lk, sk], [sk])
        p.op("dve", lambda e, o=ing, i_=tmp4.rearrange("p t g e -> p t e g"): e.reduce_sum(o, i_, AX.X), [sk], [sk])
        p.op("dve", lambda e, o=v1, i_=ing: e.reduce_max(o, i_, AX.X), [sk], [sk])
        p.tt("dve", oh1, ing, bc3(v1, 8), ALU.is_equal, [sk], [sk])
        p.stt("dve", ing2, oh1, -1e30, ing, ALU.mult, ALU.add, [sk], [sk])
        p.op("dve", lambda e, o=v2, i_=ing2: e.reduce_max(o, i_, AX.X), [sk], [sk])
        p.tt("dve", oh2, ing2, bc3(v2, 8), ALU.is_equal, [sk], [sk])
        p.tt("dve", dd, v1, v2, ALU.subtract, [sk], [sk])
        p.act(p1, dd, AF.Sigmoid, [sk], [sk])
        p.tt("dve", info[:, i0:i0 + TB, 0, 1], gp, p1, ALU.mult, [sk, "info"], ["info"])
        p.tt("dve", info[:, i0:i0 + TB, 1, 1], gp, info[:, i0:i0 + TB, 0, 1], ALU.subtract, [sk, "info"], ["info"])
        ogb = og.unsqueeze(3).to_broadcast([128, TB, 4, 8])
        E1b = E1[:, i0:i0 + TB, :].rearrange("p t (g e) -> p t g e", g=4)
        E2b = E2[:, i0:i0 + TB, :].rearrange("p t (g e) -> p t g e", g=4)
        ek = [("E1", i0 + t) for t in range(TB)] + [("E2", i0 + t) for t in range(TB)]
        p.tt("dve", E1b, ogb, oh1.unsqueeze(2).to_broadcast([128, TB, 4, 8]), ALU.mult, [sk], ek[:TB])
        p.tt("dve", E2b, ogb, oh2.unsqueeze(2).to_broadcast([128, TB, 4, 8]), ALU.mult, [sk], ek[TB:])
        p.tt("pool", Mb[b].rearrange("p (t e) -> p t e", t=TB), E1[:, i0:i0 + TB, :], E2[:, i0:i0 + TB, :], ALU.add, ek, [("Mb", b)])
        for t in range(TB):
            p.mm(psR[b][:, t * 64:t * 64 + 32], Ls, Mb[b][:, t * 32:(t + 1) * 32], True, True, ["Ls", ("Mb", b)], [("psR", b)], sig=False)
            p.mm(psR[b][:, t * 64 + 32:t * 64 + 64], onesb, Mb[b][:, t * 32:(t + 1) * 32], True, True, ["onesb", ("Mb", b)], [("psR", b)],
                 sig=(t == TB - 1))
        for t in range(TB):
            p.tt("dve", RK[:, i0 + t, :], psR[b][:, t * 64:t * 64 + 32], carry, ALU.add, [("psR", b), "carry"], [("RK", i0 + t)])
            p.tt("dve", carry, carry, psR[b][:, t * 64 + 32:t * 64 + 64], ALU.add, [("psR", b), "carry"], ["carry"])
    if MOE_STOP == 1:
        return
    p.barrier()
    mem.reset(keep)
    cnt_i = mem.f32(32).bitcast(I32)
    padded = mem.f32(32)
    pend = mem.f32(32)
    pstart = mem.f32(32)
    ones32 = mem.f32(32)
    bst = mem.f32(NB)
    be = mem.f32(NB)
    need = mem.f32(NB)
    big3 = mem.f32(max(NB, NT) * 32)
    zer = mem.f32(NB * 2)
    p.dma(bst, c.cd["bstart"][:, 0:NB], writes=["bst"])
    p.op("pool", lambda e: e.memset(ones32, 1.0), [], ["ones32"])
    p.op("pool", lambda e: e.memset(zer, 0.0), [], ["zer"])
    p.dma(c.rowinfo.rearrange("(p a) b -> p (a b)", p=128), zer, reads=["zer"], writes=["rowinfo"])
    p.ts("dve", cnt_i, carry, 127.0, None, ALU.add, None, ["carry"], ["cnt_i"])
    p.ts("dve", cnt_i, cnt_i, 7, None, ALU.arith_shift_right, None, ["cnt_i"], ["cnt_i"])
    p.ts("dve", cnt_i, cnt_i, 7, None, ALU.logical_shift_left, None, ["cnt_i"], ["cnt_i"])
    p.cp("dve", padded, cnt_i, ["cnt_i"], ["padded"])
    p.op("dve", lambda e: e.tensor_tensor_scan(pend, ones32, padded, 0.0, ALU.mult, ALU.add), ["padded", "ones32"], ["pend"])
    p.tt("dve", pstart, pend, padded, ALU.subtract, ["pend", "padded"], ["pstart"])
    cmp3 = big3[:, 0:NB * 32].rearrange("p (b e) -> p b e", e=32)
    p.tt("dve", cmp3, bst.unsqueeze(2).to_broadcast([128, NB, 32]), pend.unsqueeze(1).to_broadcast([128, NB, 32]), ALU.is_ge,
         ["bst", "pend"], ["big3"])
    p.op("dve", lambda e: e.reduce_sum(be, cmp3, AX.X), ["big3"], ["be"])
    p.ts("dve", be, be, float(NE - 1), None, ALU.min, None, ["be"], ["be"])
    p.ts("dve", be, be, 128.0, float(0 if c.precast else l * NE * 128), ALU.mult, ALU.add, ["be"], ["be"])
    p.op("dve", lambda e: e.memset(need, 1.0), [], ["need"])
    if not ALWAYS_LOAD:
        p.tt("dve", need[:, 4:NB], be[:, 4:NB], be[:, 0:NB - 4], ALU.not_equal, ["be", "need"], ["need"])
    p.ts("dve", be, be, pcol, -OOB_IDX, ALU.add, ALU.add, ["be", "pcol"], ["be"])
    p.tt("dve", be, be, need, ALU.mult, ["be", "need"], ["be"])
    p.ts("dve", widx, be, OOB_IDX, None, ALU.add, None, ["be"], ["widx"])
    pr3 = big3[:, 0:NT * 32].rearrange("p (i e) -> p i e", e=32)
    p.tt("dve", RK, RK, pstart.unsqueeze(1).to_broadcast([128, NT, 32]), ALU.add, [("RK", i) for i in range(NT)] + ["pstart"], ["RKall"])
    for k, E in enumerate((E1, E2)):
        p.tt("dve", pr3, E, RK, ALU.mult, ["RKall", "big3"] + [(("E1", "E2")[k], i) for i in range(NT)], ["big3"])
        p.op("dve", lambda e: e.reduce_sum(destf, pr3, AX.X), ["big3"], ["destf"])
        p.cp("dve", dest[k], destf, ["destf"], [("dest", k)])
    for i in range(NT):
        for k in range(2):
            p.idma(c.rowinfo, IOA(dest[k][:, i:i + 1], 0), info[:, i, k, :], None,
                   reads=[("dest", k), "info", "rowinfo"], writes=[("risc", i, k)])
    p.barrier()
    if MOE_STOP == 2:
        return
    mem.reset(keep)
    NW, NX = 4, 6
    wg = [mem.bf16(8 * 512) for _ in range(NW)]
    wu = [mem.bf16(8 * 512) for _ in range(NW)]
    wd = [mem.bf16(4 * 1024) for _ in range(NW)]
    riall = mem.f32(NB * 2).rearrange("p (b a) -> p b a", a=2)
    xg = [mem.f32(1024) for _ in range(NX)]
    xT = [mem.bf16(8 * 128).rearrange("p (j t) -> p j t", j=8) for _ in range(3)]
    sg = [mem.f32(512) for _ in range(2)]
    hT = [mem.bf16(512).rearrange("p (j t) -> p j t", j=4) for _ in range(2)]
    yb = [mem.f32(1024) for _ in range(2)]
    if c.precast:
        wgv, wuv, wdv = c.wgb[l], c.wub[l], c.wdb[l]
    else:
        wgv = c.w_gate.rearrange("l e (p j) f -> (l e p) (j f)", j=8)
        wuv = c.w_up.rearrange("l e (p j) f -> (l e p) (j f)", j=8)
        wdv = c.w_down.rearrange("l e (p j) d -> (l e p) (j d)", j=4)
    psG, psU, psY = [c.ps[2], c.ps[3]], [c.ps[4], c.ps[5]], [c.ps[6], c.ps[7]]
    WB = (NE if c.precast else DEPTH * NE) * 128 - 1
    p.dma(riall, c.rowinfo.rearrange("(b p) a -> p b a", p=128), writes=["riall"])

    def stEx(bk):
        x = bk % NX
        p.idma(xg[x], None, xin, IOA(riall.bitcast(I32)[:, bk, 0:1], 0), reads=["riall"], writes=[("xg", x)])

    def stE0(bk):
        w = bk % NW
        wi = IOA(widx[:, bk:bk + 1], 0)
        p.idma(wg[w], None, wgv, wi, reads=["widx"], writes=[("wg", w)], bounds=WB)
        p.idma(wu[w], None, wuv, wi, reads=["widx"], writes=[("wu", w)], bounds=WB)
        p.idma(wd[w], None, wdv, wi, reads=["widx"], writes=[("wd", w)], bounds=WB)

    def stE1a(bk):
        b3 = bk % 3
        x = bk % NX
        xg3 = xg[x].rearrange("p (q j) -> p j q", j=8)
        for half in range(2):
            for j in range(4):
                p.tr(psX[half][:, j * 128:(j + 1) * 128], xg3[:, half * 4 + j, :], c.ident, [("xg", x), "ident"], [("psX", half)], sig=(j == 3))
            p.cp("act" if half else "dve", xT[b3][:, half * 4:(half + 1) * 4, :], psX[half][:, :].rearrange("p (j t) -> p j t", j=4),
                 [("psX", half)], [("xT", b3)])

    def stE1(bk):
        b = bk % 2
        b3 = bk % 3
        w = bk % NW
        wg4 = wg[w].rearrange("p (j q r) -> p j r q", j=8, r=4)
        wu4 = wu[w].rearrange("p (j q r) -> p j r q", j=8, r=4)
        for (w4, ps_, wk, pk) in ((wg4, psG[b], ("wg", w), ("psG", b)), (wu4, psU[b], ("wu", w), ("psU", b))):
            for fj in range(4):
                for j in range(8):
                    p.mm(ps_[:, fj * 128:(fj + 1) * 128], w4[:, j, fj, :], xT[b3][:, j, :], j == 0, j == 7, [wk, ("xT", b3)], [pk],
                         sig=(j == 7 and fj == 3))
        p.act(sg[b], psG[b][:, :], AF.Silu, [("psG", b)], [("sg", b)])
        p.tt("dve", hT[b].rearrange("p j t -> p (j t)"), sg[b], psU[b][:, :], ALU.mult, [("sg", b), ("psU", b)], [("hT", b)])

    def stE2(bk):
        b = bk % 2
        w = bk % NW
        wd3 = wd[w].rearrange("p (j d) -> p j d", j=4)
        gate = riall[:, bk, 1:2]
        for half in range(2):
            for fj in range(4):
                p.mm(psY[half][:, :], hT[b][:, fj, :], wd3[:, fj, half * 512:(half + 1) * 512], fj == 0, fj == 3,
                     [("hT", b), ("wd", w)], [("psY", half)])
            if half == 0:
                p.act(yb[b][:, 0:512], psY[0][:, :], AF.Copy, [("psY", 0), "riall"], [("yb", b)], scale=gate)
            else:
                p.ts("dve", yb[b][:, 512:1024], psY[1][:, :], gate, None, ALU.mult, None, [("psY", 1), "riall"], [("yb", b)])
        p.dma(c.yrows[bk * 128:(bk + 1) * 128, :], yb[b], reads=[("yb", b)])

    XL = NX - 1
    for bk in range(min(XL, NB)):
        stEx(bk)
    for bk in range(min(NW, NB)):
        stE0(bk)
    stE1a(0)
    stE1a(1)
    stE1(0)
    for bk in range(NB):
        if bk + 2 < NB:
            stE1a(bk + 2)
        if bk + 1 < NB:
            stE1(bk + 1)
        stE2(bk)
        if bk + NW < NB:
            stE0(bk + NW)
        if bk + XL < NB:
            stEx(bk + XL)
    p.barrier()
    if MOE_STOP == 3:
        return
    mem.reset(keep)
    NY = 4
    y1 = [mem.f32(1024) for _ in range(NY)]
    y2 = [mem.f32(1024) for _ in range(NY)]
    xt = [mem.f32(1024) for _ in range(NY)]
    hh = [mem.f32(1024) for _ in range(2)]
    xo = [mem.f32(1024) for _ in range(2)]
    c.ln_st = [mem.f32(12) for _ in range(2)]
    c.ln_mv = [mem.f32(4) for _ in range(2)]

    def stC1(i):
        yb_ = i % NY
        p.idma(y1[yb_], None, c.yrows, IOA(dest[0][:, i:i + 1], 0), reads=[("dest", 0)], writes=[("y1", yb_)])
        p.idma(y2[yb_], None, c.yrows, IOA(dest[1][:, i:i + 1], 0), reads=[("dest", 1)], writes=[("y2", yb_)])
        p.dma(xt[yb_], xin[i * 128:(i + 1) * 128, :], writes=[("xt", yb_)])

    def stC2(i):
        b = i % 2
        yb_ = i % NY
        rows = slice(i * 128, (i + 1) * 128)
        p.stt("dve", hh[b], xt[yb_], float(ALPHA), y1[yb_], ALU.mult, ALU.add, [("xt", yb_), ("y1", yb_)], [("hh", b)])
        p.tt("pool", hh[b], hh[b], y2[yb_], ALU.add, [("hh", b), ("y2", yb_)], [("hh", b)])
        layer_norm_tile(c, hh[b], xo[b], G, Bt, b, eps)
        p.dma(xout[rows, :], xo[b], reads=[("lnout", b)], q="pool")

    for i in range(min(2, NT)):
        stC1(i)
    for i in range(NT):
        if i + 2 < NT:
            stC1(i + 2)
        stC2(i)


def make_consts(T):
    half = HD // 2
    inv = 1.0 / (10000.0 ** np.linspace(0.0, 1.0, half, dtype=np.float32))
    pos = np.arange(T, dtype=np.float32)
    ang = (pos[None, :] * inv[:, None]).astype(np.float32)
    cos = np.cos(ang).astype(np.float32)
    sin = np.sin(ang).astype(np.float32)
    cosT = np.concatenate([cos, cos], 0)
    sinT = np.concatenate([-sin, sin], 0)
    ident = np.eye(128, dtype=np.float32)
    tri64 = np.triu(np.ones((64, 64), np.float32))
    tri128 = np.triu(np.ones((128, 128), np.float32))
    SPm = min(T, 1024)
    rm = np.ones((SPm,), np.float32)
    rm[::64] = 0.0
    rmask = np.broadcast_to(rm[None], (128, SPm)).copy()
    lg = np.log(1.0 - np.power(2.0, -5.0 - np.arange(NH, dtype=np.float64)))
    i = np.arange(64, dtype=np.float64)
    sc = HD ** -0.5
    rdec = np.stack([np.exp(lg[:, None] * (i + 1)), sc * np.exp(-lg[:, None] * (i + 1)),
                     sc * np.exp(lg[:, None] * (63 - i))], 0)
    rdec = np.broadcast_to(rdec.reshape(1, -1), (128, 3 * 4 * 64)).astype(np.float32).copy()
    rel = np.broadcast_to(np.exp(lg * 64)[None], (128, 4)).astype(np.float32).copy()
    ones128 = np.full((128, 128), 1.0 / 128, np.float32)
    ones1 = np.ones((128, 128), np.float32)
    sel4 = np.zeros((4, 4, 128), np.float32)
    for h in range(4):
        sel4[h, h, :] = 1.0
    sel4 = sel4.reshape(4, 512)
    ntri128 = ((1.0 - tri128) * -30000.0).astype(np.float32)
    lstrict = np.triu(np.ones((128, 128), np.float32), 1)
    NTmax = 64
    tokid = (np.arange(NTmax, dtype=np.int32)[None, :] * 128 + np.arange(128, dtype=np.int32)[:, None]).astype(np.int32)
    bstart = np.broadcast_to((np.arange(2 * NTmax + NE, dtype=np.float32) * 128)[None], (128, 2 * NTmax + NE)).copy()
    pcol = np.arange(128, dtype=np.float32).reshape(128, 1)
    return {"lstrict": lstrict, "tokid": tokid, "bstart": bstart, "pcol": pcol, "ones1": ones1, "sel4": sel4, "ntri128": ntri128, "cosT": cosT, "sinT": sinT, "ident": ident, "tri64": tri64, "tri128": tri128, "rmask": rmask,
            "rdec": rdec, "rel": rel, "ones128": ones128}


CONST_SHAPES = lambda T: {"cosT": [128, T], "sinT": [128, T], "ident": [128, 128], "tri64": [64, 64],
                          "tri128": [128, 128], "rmask": [128, min(T, 1024)], "rdec": [128, 768], "rel": [128, 4],
                          "ones128": [128, 128], "ones1": [128, 128], "sel4": [4, 512], "ntri128": [128, 128],
                          "lstrict": [128, 128], "tokid": [128, 64], "bstart": [128, 160], "pcol": [128, 1]}


def build_program(NSEQ, T, phases=None, debug=False):
    p = Prog()
    nc = p.nc
    c = Ctx()
    c.p, c.T, c.NSEQ = p, T, NSEQ
    c.precast = PRECAST
    c.dbg = phases is not None
    NTOK = NSEQ * T
    c.x = nc.dram_tensor("x", [NTOK, D], F32, kind="ExternalInput").ap()
    c.w_in = nc.dram_tensor("w_in", [DEPTH, D, D_IN], F32, kind="ExternalInput").ap()
    c.lbl = nc.dram_tensor("hgrn_lb_logits", [DEPTH, MW], F32, kind="ExternalInput").ap()
    c.fox_bias = nc.dram_tensor("fox_fgate_bias", [DEPTH, NH], F32, kind="ExternalInput").ap()
    c.w_branch = nc.dram_tensor("w_branch", [DEPTH, 3, MW, D], F32, kind="ExternalInput").ap()
    c.w_out = nc.dram_tensor("w_out", [DEPTH, D, D], F32, kind="ExternalInput").ap()
    c.ln1_g = nc.dram_tensor("ln1_g", [DEPTH, D], F32, kind="ExternalInput").ap()
    c.ln1_b = nc.dram_tensor("ln1_b", [DEPTH, D], F32, kind="ExternalInput").ap()
    c.xmid = nc.dram_tensor("xmid", [NTOK, D], F32, kind=("ExternalOutput" if debug else "Internal")).ap()
    c.cd = {k: nc.dram_tensor(k, shp, I32 if k == "tokid" else F32, kind="ExternalInput").ap() for k, shp in CONST_SHAPES(T).items()}
    c.w_rg = nc.dram_tensor("w_router_group", [DEPTH, D, 4], F32, kind="ExternalInput").ap()
    c.w_re = nc.dram_tensor("w_router_expert", [DEPTH, D, NE], F32, kind="ExternalInput").ap()
    c.w_up = nc.dram_tensor("w_up", [DEPTH, NE, D, DFF], F32, kind="ExternalInput").ap()
    c.w_gate = nc.dram_tensor("w_gate", [DEPTH, NE, D, DFF], F32, kind="ExternalInput").ap()
    c.w_down = nc.dram_tensor("w_down", [DEPTH, NE, DFF, D], F32, kind="ExternalInput").ap()
    c.ln2_g = nc.dram_tensor("ln2_g", [DEPTH, D], F32, kind="ExternalInput").ap()
    c.ln2_b = nc.dram_tensor("ln2_b", [DEPTH, D], F32, kind="ExternalInput").ap()
    NBLK = 2 * (NTOK // 128) + NE
    c.rowinfo = nc.dram_tensor("rowinfo", [NBLK * 128, 2], F32, kind="Internal").ap()
    c.yrows = nc.dram_tensor("yrows", [NBLK * 128, D], F32, kind="Internal").ap()
    c.out = nc.dram_tensor("out", [NTOK, D], F32, kind="ExternalOutput").ap()
    c.wgb = [nc.dram_tensor("wgb%d" % l_, [NE * 128, 8 * DFF], BF16, kind="Internal").ap() for l_ in range(DEPTH)]
    c.wub = [nc.dram_tensor("wub%d" % l_, [NE * 128, 8 * DFF], BF16, kind="Internal").ap() for l_ in range(DEPTH)]
    c.wdb = [nc.dram_tensor("wdb%d" % l_, [NE * 128, 4 * D], BF16, kind="Internal").ap() for l_ in range(DEPTH)]
    kind = "ExternalOutput" if debug else "Internal"
    c.FM = [nc.dram_tensor("FM", [FM_ROWS, NTOK], F32, kind=kind).ap()]
    c.TM = nc.dram_tensor("TM", [NTOK, 1536], F32, kind=kind).ap()
    c.YT = nc.dram_tensor("YT", [1536, NTOK], BF16, kind=kind).ap()
    c.lb_d = [nc.dram_tensor("lb%d" % l, [128, 4], F32, kind="Internal").ap() for l in range(DEPTH)]
    c.mem = Mem(p)
    mem = c.mem
    c.ps = [p.raw_psum("ps%d" % i, [128, 512], F32) for i in range(8)]
    c.ident = mem.f32(128)
    c.identb = mem.bf16(128)
    c.epsb = mem.f32(1)
    c.oneb = mem.f32(1)
    mem.base = mem.off
    p.dma(c.ident, c.cd["ident"], writes=["ident"])
    p.dma(c.identb, c.cd["ident"], writes=["identb"], q="pool")
    p.op("pool", lambda e: e.memset(c.epsb, HN_EPS), [], ["epsb"])
    p.op("pool", lambda e: e.memset(c.oneb, 1.0), [], ["oneb"])
    lt = mem.f32(8)
    p.dma(lt[:, 0:4], c.lbl[0].rearrange("(h p) -> p h", p=128), writes=["lt"], allow_slow_non_contiguous=True)
    p.dma(lt[:, 4:8], c.lbl[1].rearrange("(h p) -> p h", p=128), writes=["lt"], allow_slow_non_contiguous=True)
    p.tt("dve", lt[:, 4:8], lt[:, 4:8], lt[:, 0:4], ALU.subtract, ["lt"], ["lt"])
    p.act(lt[:, 4:8], lt[:, 4:8], AF.Sigmoid, ["lt"], ["lt"])
    p.op("dve", lambda e: e.memset(lt[:, 0:4], 0.0), ["lt"], ["lt"])
    p.dma(c.lb_d[0], lt[:, 0:4], reads=["lt"])
    p.dma(c.lb_d[1], lt[:, 4:8], reads=["lt"])
    if phases is None:
        xl1 = nc.dram_tensor("xl1", [NTOK, D], F32, kind="Internal").ap()
        for l in range(DEPTH):
            xin = c.x if l == 0 else xl1
            xout = c.out if l == DEPTH - 1 else xl1
            phase_inproj(c, l, xin)
            phase_rec(c, l, "A")
            phase_fox(c, l)
            phase_rec(c, l, "C")
            phase_merge(c, l, xin, c.xmid)
            phase_moe(c, l, c.xmid, xout)
        return p.emit(), p
    L = c.dbg_layer = 0
    for ph in phases:
        if ph == "inproj":
            phase_inproj(c, L, c.x)
        elif ph == "recA":
            phase_rec(c, L, "A")
        elif ph == "recC":
            phase_rec(c, L, "C")
        elif ph == "fox":
            phase_fox(c, L)
        elif ph == "merge":
            phase_merge(c, L, c.x, c.xmid)
        elif ph == "moe":
            phase_moe(c, L, c.xmid, c.out)
    return p.emit(), p


_PROG_CACHE = {}


def kernel(x, w_in, w_branch, w_out, fox_fgate_bias, hgrn_lb_logits, ln1_g, ln1_b,
           w_router_group, w_router_expert, w_up, w_gate, w_down, ln2_g, ln2_b):
    x = np.asarray(x, dtype=np.float32)
    Bsz, T, Dm = x.shape
    NSEQ = Bsz // N_CORES
    key = (NSEQ, T)
    if key not in _PROG_CACHE:
        _PROG_CACHE[key] = build_program(NSEQ, T, phases=None, debug=False)[0]
    nc = _PROG_CACHE[key]
    f = lambda a: np.ascontiguousarray(np.asarray(a, dtype=np.float32))
    shared = {
        "w_in": f(w_in), "w_branch": f(w_branch), "w_out": f(w_out), "fox_fgate_bias": f(fox_fgate_bias),
        "hgrn_lb_logits": f(hgrn_lb_logits), "ln1_g": f(ln1_g), "ln1_b": f(ln1_b),
        "w_router_group": f(w_router_group), "w_router_expert": f(w_router_expert), "w_up": f(w_up),
        "w_gate": f(w_gate), "w_down": f(w_down), "ln2_g": f(ln2_g), "ln2_b": f(ln2_b),
    }
    shared.update(make_consts(T))
    in_maps = []
    for i in range(N_CORES):
        m = dict(shared)
        m["x"] = np.ascontiguousarray(x[i * NSEQ:(i + 1) * NSEQ].reshape(NSEQ * T, Dm))
        in_maps.append(m)
    res = run_bass_kernel_spmd(nc, in_maps, core_ids=list(range(N_CORES)))
    outs = [np.asarray(r["out"], dtype=np.float32).reshape(NSEQ, T, Dm) for r in res.results]
    return np.concatenate(outs, axis=0)
```

```python
import numpy as np
import concourse.bass as bass
import concourse.mybir as mybir
from concourse.bass_utils import run_bass_kernel_spmd

F32 = mybir.dt.float32
BF16 = mybir.dt.bfloat16
I32 = mybir.dt.int32
AF = mybir.ActivationFunctionType
ALU = mybir.AluOpType
AX = mybir.AxisListType

D = 1024
DEPTH = 2
CH = 64
HD = 128
MW = 512
NH = 4
NE = 32
DFF = 512
D_IN = 8708
ALPHA = (2 * DEPTH) ** 0.25
LN_EPS = 1e-5
HN_EPS = 1e-6
N_CORES = 8

MOE_STOP = 0
PRECAST = True
ALWAYS_LOAD = False
OOB_IDX = 1048576.0
ENGS = ("pe", "act", "dve", "pool", "sp")
NDQ = 12


class Prog:
    def __init__(self, same_sync=("act", "dve", "pool")):
        self.nc = bass.Bass("TRN2", target_bir_lowering=False)
        self.ops = {e: [] for e in ENGS}
        self.streams = (["pe", "act", "dve", "pool"] + [("dq", i) for i in range(NDQ)] + [("sq", i) for i in range(NDQ)]
                        + [("aq", i) for i in range(NDQ)])
        self.nq = {"sp": 0, "pool": 0, "act": 0}
        self.cnt = {s: 0 for s in self.streams}
        self.clock = {e: {s: 0 for s in self.streams} for e in ENGS}
        self.evclock = {}
        self.res = {}
        self.same_sync = set(same_sync)
        self.ndma = 0
        self._stack = []
        self.nops = 0
        self._bregs = {}

    def raw_sbuf(self, name, shape, dtype=F32):
        cm = self.nc.sbuf_tensor(name, list(shape), dtype)
        t = cm.__enter__()
        self._stack.append(cm)
        return t

    def raw_psum(self, name, shape, dtype=F32):
        cm = self.nc.psum_tensor(name, list(shape), dtype)
        t = cm.__enter__()
        self._stack.append(cm)
        return t

    def dram(self, name, shape, dtype=F32, kind="Internal"):
        return self.nc.dram_tensor(name, list(shape), dtype, kind=kind).ap()

    def _deps(self, eng, reads, writes):
        deps = {}

        def add(s, i):
            if s == eng and eng not in self.same_sync:
                return
            if deps.get(s, 0) < i:
                deps[s] = i

        for r in reads:
            st = self.res.get(r)
            if st and st["w"]:
                add(*st["w"])
        for w in writes:
            st = self.res.get(w)
            if st:
                if st["w"]:
                    add(*st["w"])
                for s, i in st["r"].items():
                    add(s, i)
        waits = []
        ck = self.clock[eng]
        for s, i in deps.items():
            if ck[s] >= i:
                continue
            waits.append((s, i))
            evc = self.evclock.get((s, i))
            if evc:
                for k, v in evc.items():
                    if ck[k] < v:
                        ck[k] = v
            ck[s] = i
        return waits

    def _mark(self, ev, reads, writes):
        for r in reads:
            st = self.res.setdefault(r, {"w": None, "r": {}})
            if st["r"].get(ev[0], 0) < ev[1]:
                st["r"][ev[0]] = ev[1]
        for w in writes:
            self.res[w] = {"w": ev, "r": {}}

    def op(self, eng, fn, reads=(), writes=(), sig=True):
        waits = self._deps(eng, reads, writes)
        idx = self.cnt[eng] + 1
        if sig:
            self.cnt[eng] = idx
            self.evclock[(eng, idx)] = dict(self.clock[eng])
        ev = (eng, idx)
        self._mark(ev, reads, writes)
        self.ops[eng].append((waits, fn, (eng, 1) if sig else None))
        self.nops += 1
        return ev

    def dma(self, out, in_, reads=(), writes=(), q="sp", **kw):
        k = self.nq[q]
        self.nq[q] += 1
        slot = ({"sp": "dq", "pool": "sq", "act": "aq"}[q], k % NDQ)
        idx = self.cnt[slot] + 1
        waits = self._deps(q, reads, writes)
        if idx > 1 and self.clock[q][slot] < idx - 1:
            waits.append((slot, idx - 1))
            self.clock[q][slot] = idx - 1
        self.cnt[slot] = idx
        self.evclock[(slot, idx)] = dict(self.clock[q])
        ev = (slot, idx)
        self._mark(ev, reads, writes)
        self.ops[q].append((waits, lambda e: e.dma_start(out=out, in_=in_, **kw), (slot, 16)))
        self.nops += 1
        return ev

    def idma(self, out, out_off, in_, in_off, reads=(), writes=(), bounds=None):
        k = self.nq["pool"]
        self.nq["pool"] += 1
        slot = ("sq", k % NDQ)
        idx = self.cnt[slot] + 1
        waits = self._deps("pool", reads, writes)
        if idx > 1 and self.clock["pool"][slot] < idx - 1:
            waits.append((slot, idx - 1))
            self.clock["pool"][slot] = idx - 1
        self.cnt[slot] = idx
        self.evclock[(slot, idx)] = dict(self.clock["pool"])
        ev = (slot, idx)
        self._mark(ev, reads, writes)
        if bounds is None:
            self.ops["pool"].append((waits, lambda e: e.indirect_dma_start(out, out_off, in_, in_off), (slot, 16)))
        else:
            self.ops["pool"].append((waits, lambda e: e.indirect_dma_start(out, out_off, in_, in_off, bounds_check=self._breg(e, bounds),
                                                                           oob_is_err=False), (slot, 16)))
        self.nops += 1
        return ev

    def _breg(self, e, val):
        if val not in self._bregs:
            self._bregs[val] = e.to_reg(val)
        return self._bregs[val]

    def barrier(self):
        for eng in ENGS:
            waits = []
            for s in self.streams:
                if self.cnt[s] > self.clock[eng][s]:
                    waits.append((s, self.cnt[s]))
                    self.clock[eng][s] = self.cnt[s]
            self.ops[eng].append((waits, None, None))
        self.res = {}

    def mm(self, out, lhsT, rhs, start, stop, reads, writes, sig=None):
        if sig is None:
            sig = stop
        return self.op("pe", lambda e: e.matmul(out, lhsT, rhs, start=start, stop=stop), reads, writes, sig)

    def tr(self, out, in_, ident, reads, writes, sig=True):
        return self.op("pe", lambda e: e.transpose(out, in_, ident), reads, writes, sig)

    def act(self, out, in_, func, reads, writes, bias=0.0, scale=1.0, accum_out=None):
        if accum_out is None:
            return self.op("act", lambda e: e.activation(out=out, in_=in_, func=func, bias=bias, scale=scale), reads, writes)
        return self.op("act", lambda e: e.activation(out=out, in_=in_, func=func, bias=bias, scale=scale, accum_out=accum_out), reads, writes)

    def tt(self, eng, out, in0, in1, op, reads, writes):
        return self.op(eng, lambda e: e.tensor_tensor(out, in0, in1, op), reads, writes)

    def ts(self, eng, out, in0, s1, s2, op0, op1, reads, writes):
        if s2 is None:
            return self.op(eng, lambda e: e.tensor_scalar(out, in0, s1, None, op0), reads, writes)
        return self.op(eng, lambda e: e.tensor_scalar(out, in0, s1, s2, op0, op1), reads, writes)

    def stt(self, eng, out, in0, scalar, in1, op0, op1, reads, writes):
        return self.op(eng, lambda e: e.scalar_tensor_tensor(out, in0, scalar, in1, op0, op1), reads, writes)

    def cp(self, eng, out, in_, reads, writes):
        if eng == "act":
            return self.op("act", lambda e: e.copy(out, in_), reads, writes)
        return self.op(eng, lambda e: e.tensor_copy(out, in_), reads, writes)

    def emit(self):
        nc = self.nc
        for eng in ENGS:
            pass
        self.barrier()
        sem_cms = [nc.semaphore("s_%s" % (s if isinstance(s, str) else "%s%d" % s)) for s in self.streams]
        sems = {}
        for s, cm in zip(self.streams, sem_cms):
            sems[s] = cm.__enter__()
        mult = {s: (1 if isinstance(s, str) else 16) for s in self.streams}
        blk_cm = nc.Block()
        block = blk_cm.__enter__()

        def run(e_name):
            def body(eng):
                for waits, fn, inc in self.ops[e_name]:
                    for s, i in waits:
                        eng.wait_ge(sems[s], i * mult[s])
                    if fn is None:
                        continue
                    ins = fn(eng)
                    if inc is not None:
                        ins.then_inc(sems[inc[0]], inc[1])
            return body

        block.tensor(run("pe"))
        block.scalar(run("act"))
        block.vector(run("dve"))
        block.gpsimd(run("pool"))
        block.sync(run("sp"))
        blk_cm.__exit__(None, None, None)
        for cm in reversed(sem_cms):
            cm.__exit__(None, None, None)
        for cm in reversed(self._stack):
            cm.__exit__(None, None, None)
        return nc


class Mem:
    def __init__(self, p, kbytes=190):
        self.n32 = kbytes * 256
        self.big = p.raw_sbuf("big", [128, self.n32], F32)
        self.off = 0
        self.base = 0

    def reset(self, keep=None):
        self.off = self.base if keep is None else keep

    def f32(self, n, parts=128):
        a = self.off
        self.off += (n + 7) // 8 * 8
        assert self.off <= self.n32, "SBUF overflow %d" % self.off
        return self.big[0:parts, a:a + n]

    def bf16(self, n, parts=128):
        n32 = (n + 1) // 2
        a = self.off
        self.off += (n32 + 7) // 8 * 8
        assert self.off <= self.n32, "SBUF overflow %d" % self.off
        return self.big[0:parts, a:a + n32].bitcast(BF16)


FM_AQ, FM_AF, FM_AG, FM_BQ, FM_BK, FM_CQ, FM_CK, FM_CG, FM_GA, FM_GB, FM_GC, FM_BF = (
    0, 512, 1024, 1536, 2048, 2560, 3072, 3584, 4096, 5120, 6144, 7168)
FM_ROWS = 7172
WC_AQ, WC_AF, WC_AI, WC_AG, WC_BQ, WC_BK, WC_BV, WC_BF, WC_CQ, WC_CK, WC_CV, WC_CG, WC_GA, WC_GB, WC_GC = (
    0, 512, 1024, 1536, 2048, 2560, 3072, 3584, 3588, 4100, 4612, 5124, 5636, 6660, 7684)


class Ctx:
    pass


def precast_experts(c, lw, e0, e1):
    p = c.p
    wgv_ = c.w_gate[lw].rearrange("e (p j) f -> (e p) (j f)", j=8)
    wuv_ = c.w_up[lw].rearrange("e (p j) f -> (e p) (j f)", j=8)
    wdv_ = c.w_down[lw].rearrange("e (p j) d -> (e p) (j d)", j=4)
    for e_ in range(e0, e1):
        r0 = e_ * 128
        p.dma(c.wgb[lw][r0:r0 + 128, :], wgv_[r0:r0 + 128, :], q="pool", max_dma_last_dim=4096)
        p.dma(c.wub[lw][r0:r0 + 128, :], wuv_[r0:r0 + 128, :], q="pool", max_dma_last_dim=4096)
        p.dma(c.wdb[lw][r0:r0 + 128, :], wdv_[r0:r0 + 128, :], q="pool", max_dma_last_dim=4096)


def phase_inproj(c, l, xin):
    p, mem, T = c.p, c.mem, c.T
    p.barrier()
    mem.reset()
    NT = T // 128
    NG = T // 512
    xT = mem.bf16(8 * T).rearrange("p (k t) -> p k t", k=8)
    xs = [mem.f32(D) for _ in range(2)]
    wb = [mem.bf16(8 * 128).rearrange("p (k c) -> p k c", k=8) for _ in range(4)]
    w512 = [mem.bf16(8 * 512).rearrange("p (k c) -> p k c", k=8) for _ in range(2)]
    stage = [mem.f32(T) for _ in range(2)]
    stm = [mem.f32(512) for _ in range(2)]
    tmp1 = [mem.f32(512) for _ in range(2)]
    tmp2 = [mem.f32(512) for _ in range(2)]
    c.cosT = mem.f32(T)
    c.sinT = mem.f32(T)
    p.dma(c.cosT, c.cd["cosT"], writes=["consts"])
    p.dma(c.sinT, c.cd["sinT"], writes=["consts"])
    if c.precast and l == 0:
        precast_experts(c, 0, 0, NE)
    w_l = c.w_in[l]
    wv = w_l.rearrange("(k p) c -> p k c", p=128)

    jobs = []
    for h in range(4):
        jobs.append((FM_AQ + 128 * h, WC_AQ + 128 * h, 128, "copy"))
        jobs.append((FM_AF + 128 * h, WC_AF + 128 * h, 128, "copy"))
        jobs.append((FM_AG + 128 * h, WC_AG + 128 * h, 128, "sig"))
        jobs.append((FM_BQ + 128 * h, WC_BQ + 128 * h, 128, "qscale"))
        jobs.append((FM_BK + 128 * h, WC_BK + 128 * h, 128, "copy"))
        jobs.append((FM_CQ + 128 * h, WC_CQ + 128 * h, 128, "rope"))
        jobs.append((FM_CK + 128 * h, WC_CK + 128 * h, 128, "rope"))
        jobs.append((FM_CG + 128 * h, WC_CG + 128 * h, 128, "silu"))
    for j in range(8):
        jobs.append((FM_GA + 128 * j, WC_GA + 128 * j, 128, "sig"))
        jobs.append((FM_GB + 128 * j, WC_GB + 128 * j, 128, "sig"))
        jobs.append((FM_GC + 128 * j, WC_GC + 128 * j, 128, "sig"))
    jobs.append((FM_BF, WC_BF, 4, "copy"))

    wcnt = 0
    scnt = 0
    pcnt = 0
    ecnt = 0
    for s in range(c.NSEQ):
        for i in range(NT):
            xb = xs[i % 2]
            p.dma(xb, xin[s * T + i * 128: s * T + (i + 1) * 128, :], writes=[("xs", i % 2)])
            for half in range(2):
                bank = c.ps[pcnt % 4]
                bk = ("ps", pcnt % 4)
                pcnt += 1
                for j in range(4):
                    kc = half * 4 + j
                    p.tr(bank[:, j * 128:(j + 1) * 128], xb[:, kc * 128:(kc + 1) * 128], c.ident,
                         reads=[("xs", i % 2), "ident"], writes=[bk], sig=(j == 3))
                eng = "act" if ecnt % 2 == 0 else "dve"
                ecnt += 1
                p.cp(eng, xT[:, half * 4:(half + 1) * 4, i * 128:(i + 1) * 128],
                     bank[:, :].rearrange("p (j c) -> p j c", j=4), reads=[bk], writes=[("xT", i)])
        for (row, col, nc_, kind) in jobs:
            nw = 2 if kind == "rope" else 1
            wids = []
            for v in range(nw):
                wi = wcnt % 4
                wcnt += 1
                wids.append(wi)
                if v == 0:
                    p.dma(wb[wi][:, :, 0:nc_], wv[:, :, col:col + nc_], writes=[("wb", wi)], q="pool")
                else:
                    p.dma(wb[wi][:, :, 0:64], wv[:, :, col + 64:col + 128], writes=[("wb", wi)], q="pool")
                    p.dma(wb[wi][:, :, 64:128], wv[:, :, col:col + 64], writes=[("wb", wi)], q="pool")
            sb = scnt % 2
            scnt += 1
            st = stage[sb]
            for g in range(NG):
                xr = [("xT", 4 * g + j) for j in range(4)]
                banks = []
                for v in range(nw):
                    bi = pcnt % 4
                    pcnt += 1
                    banks.append(bi)
                    for kc in range(8):
                        p.mm(c.ps[bi][0:nc_, :], wb[wids[v]][:, kc, 0:nc_], xT[:, kc, g * 512:(g + 1) * 512],
                             start=(kc == 0), stop=(kc == 7), reads=xr + [("wb", wids[v])], writes=[("ps", bi)])
                dst = st[0:nc_, g * 512:(g + 1) * 512]
                src = c.ps[banks[0]][0:nc_, :]
                rd = [("ps", banks[0])]
                wr = [("stage", sb, g)]
                if kind == "copy":
                    eng = "act" if ecnt % 2 == 0 else "dve"
                    ecnt += 1
                    p.cp(eng, dst, src, rd, wr)
                elif kind == "sig":
                    p.act(dst, src, AF.Sigmoid, rd, wr)
                elif kind == "silu":
                    p.act(dst, src, AF.Silu, rd, wr)
                elif kind == "qscale":
                    p.act(dst, src, AF.Copy, rd, wr, scale=float(HD ** -0.5))
                elif kind == "rope":
                    tb = ecnt % 2
                    ecnt += 1
                    tok = slice(g * 512, (g + 1) * 512)
                    p.tt("dve", tmp1[tb], src, c.cosT[:, tok], ALU.mult, rd + ["consts"], [("tmp1", tb)])
                    p.tt("dve", tmp2[tb], c.ps[banks[1]][:, :], c.sinT[:, tok], ALU.mult,
                         [("ps", banks[1]), "consts"], [("tmp2", tb)])
                    p.tt("pool", dst, tmp1[tb], tmp2[tb], ALU.add, [("tmp1", tb), ("tmp2", tb)], wr)
            p.dma(c.FM[l % 1][row:row + nc_, s * T:(s + 1) * T], st[0:nc_, :],
                  reads=[("stage", sb, g) for g in range(NG)])
        for vi, col in enumerate((WC_AI, WC_BV, WC_CV)):
            wi = vi % 2
            p.dma(w512[wi], wv[:, :, col:col + 512], writes=[("w512", wi)], q="pool")
            for i in range(NT):
                bi = pcnt % 4
                pcnt += 1
                for kc in range(8):
                    p.mm(c.ps[bi][:, :], xT[:, kc, i * 128:(i + 1) * 128], w512[wi][:, kc, :],
                         start=(kc == 0), stop=(kc == 7), reads=[("xT", i), ("w512", wi)], writes=[("ps", bi)])
                sb = scnt % 2
                scnt += 1
                eng = "act" if ecnt % 2 == 0 else "dve"
                ecnt += 1
                p.cp(eng, stm[sb], c.ps[bi][:, :], [("ps", bi)], [("stm", sb)])
                p.dma(c.TM[s * T + i * 128: s * T + (i + 1) * 128, vi * 512:(vi + 1) * 512], stm[sb],
                      reads=[("stm", sb)])


def phase_rec(c, l, br):
    p, mem, T = c.p, c.mem, c.T
    p.barrier()
    mem.reset()
    SP = min(T, 1024)
    NSP = T // SP
    NCH = SP // 64
    isA = br == "A"
    if c.precast and l + 1 < DEPTH and not c.dbg:
        precast_experts(c, l + 1, 0 if isA else 2 * NE // 3, NE // 3 if isA else NE)
    qrow, krow, grow = (FM_AQ, FM_AF, FM_AG) if isA else (FM_CQ, FM_CK, FM_CG)
    vcol = 0 if isA else 1024
    yrow = 0 if isA else 1024
    FM = c.FM[0]
    qe = [[mem.bf16(SP) for _ in range(4)] for _ in range(2)]
    ke = [[mem.bf16(SP) for _ in range(4)] for _ in range(2)]
    kh = [[mem.bf16(SP) for _ in range(4)] for _ in range(2)]
    V = [mem.bf16(NCH * 512, parts=64).rearrange("p (c f) -> p c f", c=NCH) for _ in range(2)]
    elast = [mem.f32(4 * NCH).rearrange("p (h c) -> p h c", h=4) for _ in range(2)]
    Tm = [[mem.f32(SP) for _ in range(6)] for _ in range(2)]
    S = mem.f32(512)
    Sb = [mem.bf16(512) for _ in range(2)]
    scnt = [0]
    sm = [mem.bf16(256, parts=64) for _ in range(2)]
    kt = [mem.bf16(512, parts=64) for _ in range(2)]
    tri = mem.f32(64, parts=64)
    rmask = mem.f32(SP)
    ones = mem.bf16(128)
    osb = [mem.f32(512) for _ in range(4)]
    gt = [mem.f32(512) for _ in range(2)]
    w1 = [mem.f32(512) for _ in range(2)]
    w2 = [mem.f32(512) for _ in range(2)]
    wbf = [mem.bf16(512) for _ in range(2)]
    yb = [mem.bf16(512) for _ in range(2)]
    lbt = mem.f32(4)
    omt = mem.f32(4)
    rdec = mem.f32(3 * 4 * 64).rearrange("p (a h i) -> p a h i", a=3, h=4)
    rel = mem.f32(4)
    psO = [c.ps[h] for h in range(4)]
    psS32, psT32, psN, psM = c.ps[4], c.ps[5], c.ps[6], c.ps[7]
    psS = [psS32[0:64, 0:256], psS32[0:64, 256:512]]
    psTb = psT32[:, :].bitcast(BF16)
    psT = [psTb[0:64, 0:512], psTb[0:64, 512:1024]]

    p.dma(tri, c.cd["tri64"], writes=["tri"])
    p.dma(rmask, c.cd["rmask"][:, 0:SP], writes=["rmask"])
    p.dma(ones, c.cd["ones128"], writes=["ones"], q="pool")
    if isA:
        p.dma(lbt, c.lb_d[l], writes=["lbt"])
        p.ts("dve", omt, lbt, -1.0, 1.0, ALU.mult, ALU.add, ["lbt"], ["omt"])
    else:
        p.dma(rdec.rearrange("p a h i -> p (a h i)"), c.cd["rdec"], writes=["rdec"])
        p.dma(rel, c.cd["rel"], writes=["rel"])
    spans = [(s, sp) for s in range(c.NSEQ) for sp in range(NSP)]
    v3 = lambda a: a.rearrange("p (c i) -> p c i", i=64)

    def prep_gen(n):
        s, sp = spans[n]
        bs = n % 2
        t0 = s * T + sp * SP
        tok = slice(t0, t0 + SP)
        p.dma(V[bs], c.TM[t0:t0 + SP, vcol:vcol + 512].rearrange("(c p) f -> p c f", p=64), writes=[("V", bs)], q="pool")
        yield
        for h in range(4):
            hb = h % 2
            T0, T1, T2, T3, T4, T5 = Tm[hb]
            k0, k1, k2, k3, k4, k5 = [("Tm", hb, j) for j in range(6)]
            qk, kk_, hk, ek = ("qe", bs, h), ("ke", bs, h), ("kh", bs, h), ("elast", bs)
            if isA:
                p.dma(T0, FM[krow + 128 * h: krow + 128 * h + 128, tok], writes=[k0])
                p.dma(T3, FM[qrow + 128 * h: qrow + 128 * h + 128, tok], writes=[k3])
                p.act(T0, T0, AF.Sigmoid, [k0], [k0])
                yield
                p.act(T0, T0, AF.Identity, [k0, "omt", "lbt"], [k0], bias=lbt[:, h:h + 1], scale=omt[:, h:h + 1])
                yield
                p.act(T1, T0, AF.Ln, [k0], [k1])
                yield
                p.op("dve", lambda e, T2=T2, T1=T1: e.tensor_tensor_scan(T2, rmask, T1, 0.0, ALU.mult, ALU.add),
                     [k1, "rmask"], [k2])
                yield
                p.act(T0, T0, AF.Identity, [k0], [k0], bias=c.oneb, scale=-1.0)
                yield
                p.act(T1, T2, AF.Exp, [k2], [k1])
                yield
                p.tt("dve", qe[bs][h], T3, T1, ALU.mult, [k3, k1], [qk])
                yield
                p.act(T4, T2, AF.Exp, [k2], [k4], scale=-1.0)
                yield
                p.tt("pool", ke[bs][h], T0, T4, ALU.mult, [k0, k4], [kk_])
                yield
                p.act(elast[bs][:, h, :], v3(T2)[:, :, 63], AF.Exp, [k2], [ek])
                p.tt("dve", v3(T5), v3(T2)[:, :, 63:64].to_broadcast([128, NCH, 64]), v3(T2), ALU.subtract, [k2], [k5])
                yield
                p.act(T5, T5, AF.Exp, [k5], [k5])
                yield
                p.tt("pool", kh[bs][h], T0, T5, ALU.mult, [k0, k5], [hk])
                yield
            else:
                bc = lambda a: a.unsqueeze(1).to_broadcast([128, NCH, 64])
                p.dma(T3, FM[qrow + 128 * h: qrow + 128 * h + 128, tok], writes=[k3])
                p.dma(T0, FM[krow + 128 * h: krow + 128 * h + 128, tok], writes=[k0])
                p.tt("dve", v3(qe[bs][h]), v3(T3), bc(rdec[:, 0, h, :]), ALU.mult, [k3, "rdec"], [qk])
                yield
                p.tt("dve", v3(ke[bs][h]), v3(T0), bc(rdec[:, 1, h, :]), ALU.mult, [k0, "rdec"], [kk_])
                yield
                p.tt("pool", v3(kh[bs][h]), v3(T0), bc(rdec[:, 2, h, :]), ALU.mult, [k0, "rdec"], [hk])
                p.cp("dve", elast[bs][:, h, :], rel[:, h:h + 1].to_broadcast([128, NCH]), ["rel"], [ek])
                yield

    def post_gen(gtok, g):
        for h in range(4):
            b = (g * 4 + h) % 2
            p.dma(gt[b], FM[grow + 128 * h: grow + 128 * h + 128, gtok], writes=[("gt", b)])
            oc = osb[h]
            if not isA:
                p.cp("pool", wbf[b], osb[h], [("osb", h)], [("wbf", b)])
                p.mm(psM[:, :], ones, wbf[b], True, True, ["ones", ("wbf", b)], ["psM"])
                yield
                p.tt("dve", osb[h], osb[h], psM[:, :], ALU.subtract, [("osb", h), "psM"], [("osb", h)])
                yield
            p.act(wbf[b], oc, AF.Square, [("osb", h)], [("wbf", b)])
            p.mm(psM[:, :], ones, wbf[b], True, True, ["ones", ("wbf", b)], ["psM"])
            yield
            p.act(w1[b], psM[:, :], AF.Ln, ["psM", "epsb"], [("w1", b)], bias=c.epsb)
            yield
            p.act(w1[b], w1[b], AF.Exp, [("w1", b)], [("w1", b)], scale=-0.5)
            yield
            p.tt("dve", w2[b], oc, w1[b], ALU.mult, [("osb", h), ("w1", b)], [("w2", b)])
            yield
            p.tt("pool", yb[b], w2[b], gt[b], ALU.mult, [("w2", b), ("gt", b)], [("yb", b)])
            p.dma(c.YT[yrow + 128 * h: yrow + 128 * h + 128, gtok], yb[b], reads=[("yb", b)], q="pool")
            yield

    posts = []

    def drain(gens, k):
        for _ in range(k):
            while gens:
                try:
                    next(gens[0])
                    break
                except StopIteration:
                    gens.pop(0)

    def exhaust(gens):
        while gens:
            for _ in gens[0]:
                pass
            gens.pop(0)

    preps = [prep_gen(0)]
    exhaust(preps)
    gcount = 0
    for n, (s, sp) in enumerate(spans):
        bs = n % 2
        t0 = s * T + sp * SP
        if sp == 0:
            p.op("pool", lambda e: e.memset(S, 0.0), ["S"], ["S"])
            p.op("pool", lambda e, o=Sb[scnt[0] % 2]: e.memset(o, 0.0), [], [("Sb", scnt[0] % 2)])
        preps = [prep_gen(n + 1)] if n + 1 < len(spans) else []
        Q, KE, KH, VV, EL = qe[bs], ke[bs], kh[bs], V[bs], elast[bs]

        def stA1(ch):
            cs = slice(ch * 64, ch * 64 + 64)
            b2 = ch % 2
            for h in range(4):
                p.mm(psS[b2][:, h * 64:(h + 1) * 64], KE[h][:, cs], Q[h][:, cs], True, True,
                     [("ke", bs, h), ("qe", bs, h)], [("psS", b2)], sig=(h == 3))
            p.tt("dve", sm[b2].rearrange("p (h t) -> p h t", h=4), psS[b2].rearrange("p (h t) -> p h t", h=4),
                 tri.unsqueeze(1).to_broadcast([64, 4, 64]), ALU.mult, [("psS", b2), "tri"], [("sm", b2)])
            for h in range(4):
                p.tr(psT[b2][:, h * 128:(h + 1) * 128], KH[h][:, cs], c.identb, [("kh", bs, h), "identb"], [("psT", b2)], sig=(h == 3))
            p.cp("act", kt[b2], psT[b2], [("psT", b2)], [("kt", b2)])

        def stA2(ch):
            b2 = ch % 2
            gs = slice((ch % 8) * 64, (ch % 8) * 64 + 64)
            for h in range(4):
                hs = slice(128 * h, 128 * h + 128)
                p.mm(psN[:, hs], kt[b2][:, hs], VV[:, ch, hs], True, True, [("kt", b2), ("V", bs)], ["psN"], sig=(h == 3))
            for h in range(4):
                hs = slice(128 * h, 128 * h + 128)
                p.mm(psO[h][:, gs], VV[:, ch, hs], sm[b2][:, h * 64:(h + 1) * 64], True, False,
                     [("V", bs), ("sm", b2)], [("psO", h)], sig=False)

        def stB(ch):
            nonlocal gcount
            cs = slice(ch * 64, ch * 64 + 64)
            g = ch // 8
            gs = slice((ch % 8) * 64, (ch % 8) * 64 + 64)
            sbi = scnt[0] % 2
            scnt[0] += 1
            for h in range(4):
                hs = slice(128 * h, 128 * h + 128)
                p.mm(psO[h][:, gs], Sb[sbi][:, hs], Q[h][:, cs], False, True, [("Sb", sbi), ("qe", bs, h)], [("psO", h)], sig=True)
            S3 = S.rearrange("p (h v) -> p h v", h=4)
            p.tt("dve", S3, S3, EL[:, :, ch:ch + 1].to_broadcast([128, 4, 128]), ALU.mult, ["S", ("elast", bs)], ["S"])
            p.tt("dve", S, S, psN[:, :], ALU.add, ["S", "psN"], ["S"])
            p.cp("act", Sb[1 - sbi], S, ["S"], [("Sb", 1 - sbi)])
            if ch % 8 == 7:
                exhaust(posts)
                for h in range(4):
                    p.cp("act" if h % 2 else "dve", osb[h], psO[h][:, :], [("psO", h)], [("osb", h)])
                posts.append(post_gen(slice(t0 + g * 512, t0 + g * 512 + 512), gcount))
                gcount += 1

        stA1(0)
        stA2(0)
        for ch in range(NCH):
            if ch + 1 < NCH:
                stA1(ch + 1)
            stB(ch)
            if ch + 1 < NCH:
                stA2(ch + 1)
            drain(posts, 4)
            drain(preps, 5)
        exhaust(preps)
    exhaust(posts)


def phase_fox(c, l):
    p, mem, T = c.p, c.mem, c.T
    p.barrier()
    mem.reset()
    NT = T // 128
    NG = T // 512
    FM = c.FM[0]
    if c.precast and l + 1 < DEPTH and not c.dbg:
        precast_experts(c, l + 1, NE // 3, 2 * NE // 3)
    sd = float(HD ** 0.5)
    qT = mem.bf16(T)
    kT = mem.bf16(T)
    V = mem.bf16(NT * 128).rearrange("p (j v) -> p j v", j=NT)
    B = mem.f32(T)
    cum1 = mem.f32(T, parts=4)
    cum2 = mem.f32(T, parts=4)
    ones4 = mem.f32(T, parts=4)
    negc = mem.f32(NT * 4).rearrange("p (j h) -> p j h", h=4)
    sel = mem.f32(4 * 128, parts=4).rearrange("p (h m) -> p h m", h=4)
    fb = mem.f32(1, parts=4)
    tri = mem.f32(128)
    onesb = mem.bf16(128)
    sq = [mem.bf16(512) for _ in range(2)]
    tmp = [mem.f32(512) for _ in range(4)]
    PT = [mem.bf16(512) for _ in range(5)]
    kmaxs = mem.f32(NG)
    kterm = mem.f32(1)
    rl = [mem.f32(512) for _ in range(2)]
    yb = [mem.bf16(512) for _ in range(2)]
    psA = [c.ps[0], c.ps[1], c.ps[2], c.ps[7]]
    psO = [c.ps[3], c.ps[4]]
    psL = [c.ps[5], c.ps[6]]
    psX = [c.ps[7], c.ps[7]]

    p.dma(tri, c.cd["ntri128"], writes=["tri"])
    p.dma(onesb, c.cd["ones1"], writes=["onesb"], q="pool")
    p.dma(sel.rearrange("p h m -> p (h m)"), c.cd["sel4"], writes=["sel"])
    p.dma(fb, c.fox_bias[l].rearrange("(h o) -> h o", o=1), writes=["fb"])
    p.op("pool", lambda e: e.memset(ones4, 1.0), [], ["ones4"])
    acnt = 0
    xcnt = 0
    gcnt = 0
    pcnt = 0
    for s in range(c.NSEQ):
        tok = slice(s * T, (s + 1) * T)
        p.dma(cum1, FM[FM_BF:FM_BF + 4, tok], writes=["cum1"])
        p.act(cum1, cum1, AF.Sigmoid, ["cum1", "fb"], ["cum1"], bias=fb)
        p.act(cum1, cum1, AF.Ln, ["cum1"], ["cum1"])
        p.op("dve", lambda e: e.tensor_tensor_scan(cum2, ones4, cum1, 0.0, ALU.mult, ALU.add), ["cum1", "ones4"], ["cum2"])
        xb = 0
        xcnt += 1
        for j in range(NT):
            p.tr(psX[xb][:, j * 4:(j + 1) * 4], cum2[:, j * 128:(j + 1) * 128], c.ident[0:4, 0:4],
                 ["cum2", "ident"], [("psA", 3)], sig=(j == NT - 1))
        p.act(negc.rearrange("p j h -> p (j h)"), psX[xb][:, 0:NT * 4], AF.Copy, [("psA", 3)], ["negc"], scale=-1.0)
        for h in range(4):
            hr = slice(128 * h, 128 * h + 128)
            p.dma(qT, FM[FM_BQ + 128 * h: FM_BQ + 128 * h + 128, tok], writes=["qT"], q="pool")
            p.dma(kT, FM[FM_BK + 128 * h: FM_BK + 128 * h + 128, tok], writes=["kT"], q="pool")
            p.dma(V, c.TM[tok, 512 + 128 * h: 512 + 128 * h + 128].rearrange("(j p) v -> p j v", p=128), writes=["V"], q="pool")
            for g in range(NG):
                gs = slice(g * 512, (g + 1) * 512)
                b = acnt % 2
                acnt += 1
                p.act(sq[b], kT[:, gs], AF.Square, ["kT"], [("sq", b)])
                p.mm(psA[b][:, :], onesb, sq[b], True, True, ["onesb", ("sq", b)], [("psA", b)])
                p.op("dve", lambda e, o=kmaxs[:, g:g + 1], i=psA[b][:, :]: e.reduce_max(o, i, AX.X), [("psA", b)], ["kmaxs"])
            p.op("dve", lambda e: e.reduce_max(kterm, kmaxs, AX.X), ["kmaxs"], ["kterm"])
            p.ts("dve", kterm, kterm, -0.5 / sd, None, ALU.mult, None, ["kterm"], ["kterm"])
            for g in range(NG):
                gs = slice(g * 512, (g + 1) * 512)
                b = acnt % 2
                acnt += 1
                xb = 0
                xcnt += 1
                p.act(sq[b], qT[:, gs], AF.Square, ["qT"], [("sq", b)])
                p.mm(psA[b][:, :], onesb, sq[b], True, True, ["onesb", ("sq", b)], [("psA", b)])
                p.act(tmp[b], psA[b][:, :], AF.Copy, [("psA", b)], [("tmp", b)], scale=-0.5 * sd)
                p.mm(psX[xb][:, :], sel[:, h, :], cum2[:, gs], True, True, ["sel", "cum2"], [("psA", 3)])
                p.stt("dve", B[:, gs], psX[xb][:, :], kterm, tmp[b], ALU.add, ALU.add,
                      [("psA", 3), "kterm", ("tmp", b)], [("B", g)])
            blocks = []
            for g in range(NG):
                nk = 4 * g + 4
                for j in range(nk):
                    blocks.append((g, j, nk))
            obs = {}
            for g in range(NG):
                obs[g] = gcnt % 2
                gcnt += 1
            st1 = {}

            def stage1(n):
                nonlocal acnt, pcnt
                g, j, nk = blocks[n]
                jj = j - 4 * g
                t0 = max(0, jj) * 128
                qs = slice(g * 512 + t0, (g + 1) * 512)
                ls = slice(t0, 512)
                b = acnt % 4
                acnt += 1
                pb = pcnt % 5
                pcnt += 1
                p.mm(psA[b][:, ls], kT[:, j * 128:(j + 1) * 128], qT[:, qs], True, True, ["kT", "qT"], [("psA", b)])
                p.tt("dve", tmp[b][:, ls], psA[b][:, ls], B[:, qs], ALU.add, [("psA", b), ("B", g)], [("tmp", b)])
                if jj >= 0:
                    p.tt("pool", tmp[b][:, t0:t0 + 128], tmp[b][:, t0:t0 + 128], tri, ALU.add, [("tmp", b), "tri"], [("tmp", b)])
                p.act(PT[pb][:, ls], tmp[b][:, ls], AF.Exp, [("tmp", b), "negc"], [("PT", pb)], bias=negc[:, j, h:h + 1])
                st1[n] = (pb, ls)

            def stage2(n):
                g, j, nk = blocks[n]
                pb, ls = st1.pop(n)
                ob = obs[g]
                p.mm(psO[ob][:, ls], V[:, j, :], PT[pb][:, ls], j == 0, j == nk - 1, ["V", ("PT", pb)], [("psO", ob)])
                p.mm(psL[ob][:, ls], onesb, PT[pb][:, ls], j == 0, j == nk - 1, ["onesb", ("PT", pb)], [("psL", ob)])
                if j == nk - 1:
                    p.act(rl[ob], psL[ob][:, :], AF.Ln, [("psL", ob)], [("rl", ob)])
                    p.act(rl[ob], rl[ob], AF.Exp, [("rl", ob)], [("rl", ob)], scale=-1.0)
                    p.tt("dve", yb[ob], psO[ob][:, :], rl[ob], ALU.mult, [("psO", ob), ("rl", ob)], [("yb", ob)])
                    p.dma(c.YT[512 + 128 * h: 512 + 128 * h + 128, s * T + g * 512: s * T + (g + 1) * 512], yb[ob],
                          reads=[("yb", ob)])

            LA = 3
            for n in range(min(LA, len(blocks))):
                stage1(n)
            for n in range(len(blocks)):
                if n + LA < len(blocks):
                    stage1(n + LA)
                stage2(n)


def layer_norm_tile(c, hh, outt, G, Bt, tagb, eps_ap):
    p = c.p
    st, mv = c.ln_st[tagb], c.ln_mv[tagb]
    hk, ok = ("hh", tagb), ("lnout", tagb)
    p.op("dve", lambda e: e.bn_stats(st[:, 0:6], hh[:, 0:512]), [hk], [("lnst", tagb)])
    p.op("dve", lambda e: e.bn_stats(st[:, 6:12], hh[:, 512:1024]), [hk], [("lnst", tagb)])
    p.op("dve", lambda e: e.bn_aggr(mv[:, 0:2], st[:, 0:12]), [("lnst", tagb)], [("lnmv", tagb)])
    p.act(mv[:, 2:3], mv[:, 1:2], AF.Sqrt, [("lnmv", tagb), "lneps"], [("lnmv", tagb)], bias=eps_ap)
    p.op("dve", lambda e: e.reciprocal(mv[:, 2:3], mv[:, 2:3]), [("lnmv", tagb)], [("lnmv", tagb)])
    p.ts("dve", mv[:, 3:4], mv[:, 0:1], -1.0, None, ALU.mult, None, [("lnmv", tagb)], [("lnmv", tagb)])
    p.act(outt, hh, AF.Identity, [hk, ("lnmv", tagb)], [ok], bias=mv[:, 3:4])
    p.stt("dve", outt, outt, mv[:, 2:3], G, ALU.mult, ALU.mult, [ok, ("lnmv", tagb), "lnG"], [ok])
    p.tt("pool", outt, outt, Bt, ALU.add, [ok, "lnG"], [ok])


def phase_merge(c, l, xin, xout):
    p, mem, T = c.p, c.mem, c.T
    p.barrier()
    mem.reset()
    NTOK = c.NSEQ * T
    FM = c.FM[0]
    wbr = mem.bf16(3 * 4 * 1024).rearrange("p (b k c) -> p b k c", b=3, k=4)
    wo = mem.bf16(8 * 1024).rearrange("p (k c) -> p k c", k=8)
    G = mem.f32(1024)
    Bt = mem.f32(1024)
    eps = mem.f32(1)
    yT = [mem.bf16(3 * 4 * 512).rearrange("p (b k t) -> p b k t", b=3, k=4) for _ in range(2)]
    gts = [mem.f32(512) for _ in range(6)]
    tt_ = [mem.f32(512) for _ in range(6)]
    mT = [mem.bf16(8 * 512).rearrange("p (k t) -> p k t", k=8) for _ in range(2)]
    xt = [mem.f32(1024) for _ in range(2)]
    hh = [mem.f32(1024) for _ in range(2)]
    xo = [mem.f32(1024) for _ in range(2)]
    c.ln_st = [mem.f32(12) for _ in range(2)]
    c.ln_mv = [mem.f32(4) for _ in range(2)]
    p.dma(wbr.rearrange("p b k c -> p (b k) c"), c.w_branch[l].rearrange("b (k p) c -> p (b k) c", p=128), writes=["wbr"], q="pool")
    p.dma(wo, c.w_out[l].rearrange("(k p) c -> p k c", p=128), writes=["wo"], q="pool")
    p.dma(G, c.ln1_g[l].partition_broadcast(128), writes=["lnG"])
    p.dma(Bt, c.ln1_b[l].partition_broadcast(128), writes=["lnG"])
    p.op("pool", lambda e: e.memset(eps, LN_EPS), [], ["lneps"])
    cnts = {"g": 0, "pc": 0, "t": 0}
    NGR = NTOK // 512

    def branch_units(g):
        gtok = slice(g * 512, (g + 1) * 512)
        yb = g % 2
        p.dma(yT[yb].rearrange("p b k t -> p (b k) t"), c.YT[:, gtok].rearrange("(bk p) t -> p bk t", p=128), writes=[("yT", yb)])
        for cc in range(8):
            ts_ = []
            for br in range(3):
                bi = cnts["pc"] % 6
                cnts["pc"] += 1
                gi = cnts["g"] % 6
                cnts["g"] += 1
                for kc in range(4):
                    p.mm(c.ps[bi][:, :], wbr[:, br, kc, cc * 128:(cc + 1) * 128], yT[yb][:, br, kc, :], kc == 0, kc == 3,
                         ["wbr", ("yT", yb)], [("ps", bi)])
                row = FM_GA + br * 1024 + cc * 128
                p.dma(gts[gi], FM[row:row + 128, gtok], writes=[("gts", gi)])
                p.tt("dve", tt_[gi], c.ps[bi][:, :], gts[gi], ALU.mult, [("ps", bi), ("gts", gi)], [("tt", gi)])
                ts_.append(gi)
            a_, b_, d_ = ts_
            p.tt("pool", tt_[a_], tt_[a_], tt_[b_], ALU.add, [("tt", a_), ("tt", b_)], [("tt", a_)])
            p.tt("pool", mT[yb][:, cc, :], tt_[a_], tt_[d_], ALU.add, [("tt", a_), ("tt", d_)], [("mT", yb, cc)])
            yield

    def out_units(g):
        yb = g % 2
        for i in range(4):
            tb = cnts["t"] % 2
            cnts["t"] += 1
            rows = slice(g * 512 + i * 128, g * 512 + (i + 1) * 128)
            p.dma(xt[tb], xin[rows, :], writes=[("xt", tb)])
            for half in range(2):
                for cc in range(8):
                    p.mm(c.ps[6 + half][:, :], mT[yb][:, cc, i * 128:(i + 1) * 128], wo[:, cc, half * 512:(half + 1) * 512],
                         cc == 0, cc == 7, [("mT", yb, cc), "wo"], [("ps", 6 + half)])
                p.stt("dve", hh[tb][:, half * 512:(half + 1) * 512], xt[tb][:, half * 512:(half + 1) * 512], float(ALPHA),
                      c.ps[6 + half][:, :], ALU.mult, ALU.add, [("xt", tb), ("ps", 6 + half)], [("hh", tb)])
            layer_norm_tile(c, hh[tb], xo[tb], G, Bt, tb, eps)
            p.dma(xout[rows, :], xo[tb], reads=[("lnout", tb)], q="pool")
            yield

    for _ in branch_units(0):
        pass
    for g in range(NGR):
        bu = branch_units(g + 1) if g + 1 < NGR else iter(())
        ou = out_units(g)
        for k in range(4):
            next(bu, None)
            next(bu, None)
            next(ou, None)
        for _ in bu:
            pass


def phase_moe(c, l, xin, xout):
    p, mem, T = c.p, c.mem, c.T
    p.barrier()
    mem.reset()
    N = c.NSEQ * T
    NT = N // 128
    NB = 2 * NT + NE
    NR = NB * 128
    IOA = bass.IndirectOffsetOnAxis
    E1 = mem.f32(NT * 32).rearrange("p (i e) -> p i e", e=32)
    E2 = mem.f32(NT * 32).rearrange("p (i e) -> p i e", e=32)
    RK = mem.f32(NT * 32).rearrange("p (i e) -> p i e", e=32)
    info = mem.f32(NT * 4).rearrange("p (i k a) -> p i k a", k=2, a=2)
    dest = [mem.f32(NT).bitcast(I32) for _ in range(2)]
    destf = mem.f32(NT)
    widx = mem.f32(NB).bitcast(I32)
    carry = mem.f32(32)
    wr = mem.f32(8 * 36).rearrange("p (k c) -> p k c", k=8)
    Ls = mem.bf16(128)
    onesb = mem.bf16(128)
    G = mem.f32(1024)
    Bt = mem.f32(1024)
    eps = mem.f32(1)
    pcol = mem.f32(1)
    keep = mem.off
    xt = [mem.f32(1024) for _ in range(4)]
    x1T = [mem.f32(8 * 128).rearrange("p (k t) -> p k t", k=8) for _ in range(4)]
    lg = [mem.f32(4 * 36) for _ in range(2)]
    smb = [mem.f32(352) for _ in range(2)]
    Mb = [mem.bf16(4 * 32) for _ in range(2)]
    p.dma(wr[:, :, 0:4], c.w_rg[l].rearrange("(k p) c -> p k c", p=128), writes=["wr"], allow_slow_non_contiguous=True)
    p.dma(wr[:, :, 4:36], c.w_re[l].rearrange("(k p) c -> p k c", p=128), writes=["wr"], allow_slow_non_contiguous=True)
    p.dma(Ls, c.cd["lstrict"], writes=["Ls"], q="pool")
    p.dma(onesb, c.cd["ones1"], writes=["onesb"], q="pool")
    p.dma(G, c.ln2_g[l].partition_broadcast(128), writes=["lnG"])
    p.dma(Bt, c.ln2_b[l].partition_broadcast(128), writes=["lnG"])
    p.dma(pcol, c.cd["pcol"], writes=["pcol"])
    p.op("pool", lambda e: e.memset(eps, LN_EPS), [], ["lneps"])
    p.op("pool", lambda e: e.memset(carry, 0.0), [], ["carry"])
    p.dma(info.bitcast(I32)[:, :, 0, 0], c.cd["tokid"][:, 0:NT], writes=["info"], allow_slow_non_contiguous=True)
    p.dma(info.bitcast(I32)[:, :, 1, 0], c.cd["tokid"][:, 0:NT], writes=["info"], allow_slow_non_contiguous=True)
    psX = [c.ps[0], c.ps[1]]
    psLg, psR = [c.ps[2], c.ps[3]], [c.ps[4], c.ps[5]]
    TB = 4
    for bt in range(NT // TB):
        b = bt % 2
        i0 = bt * TB
        S_ = smb[b]
        sk = ("sm", b)
        off = [0]

        def carve(n):
            a_ = S_[:, off[0]:off[0] + n]
            off[0] += n
            return a_
        gmax, gsum, gp, v1, v2, dd, p1 = [carve(4) for _ in range(7)]
        og = carve(16).rearrange("p (t g) -> p t g", t=4)
        ge = carve(16).rearrange("p (t g) -> p t g", t=4)
        ing = carve(32).rearrange("p (t e) -> p t e", t=4)
        ing2 = carve(32).rearrange("p (t e) -> p t e", t=4)
        oh1 = carve(32).rearrange("p (t e) -> p t e", t=4)
        oh2 = carve(32).rearrange("p (t e) -> p t e", t=4)
        tmp4 = carve(128).rearrange("p (t g e) -> p t g e", t=4, g=4)
        for t in range(TB):
            i = i0 + t
            xb = i % 4
            p.dma(xt[xb], xin[i * 128:(i + 1) * 128, :], writes=[("xt", xb)])
            for half in range(2):
                for j in range(4):
                    kc = half * 4 + j
                    p.tr(psX[half][:, j * 128:(j + 1) * 128], xt[xb][:, kc * 128:(kc + 1) * 128], c.ident, [("xt", xb), "ident"],
                         [("psX", half)], sig=(j == 3))
                p.cp("act" if half else "dve", x1T[xb][:, half * 4:(half + 1) * 4, :], psX[half][:, :].rearrange("p (j t) -> p j t", j=4),
                     [("psX", half)], [("x1T", xb)])
            for kc in range(8):
                p.mm(psLg[b][:, t * 36:(t + 1) * 36], x1T[xb][:, kc, :], wr[:, kc, :], kc == 0, kc == 7, [("x1T", xb), "wr"], [("psLg", b)])
        p.cp("act", lg[b], psLg[b][:, 0:TB * 36], [("psLg", b)], [("lg", b)])
        L3 = lg[b].rearrange("p (t c) -> p t c", t=TB)
        Lg = L3[:, :, 0:4]
        Le = L3[:, :, 4:36].rearrange("p t (g e) -> p t g e", g=4)
        lk = ("lg", b)
        bc3 = lambda a_, n: a_.unsqueeze(2).to_broadcast([128, TB, n])
        p.op("dve", lambda e, o=gmax, i_=Lg: e.reduce_max(o, i_, AX.X), [lk], [sk])
        p.tt("dve", og, Lg, bc3(gmax, 4), ALU.is_equal, [lk, sk], [sk])
        p.tt("dve", ge, Lg, bc3(gmax, 4), ALU.subtract, [lk, sk], [sk])
        p.act(ge, ge, AF.Exp, [sk], [sk])
        p.op("dve", lambda e, o=gsum, i_=ge: e.reduce_sum(o, i_, AX.X), [sk], [sk])
        p.op("dve", lambda e, o=gp, i_=gsum: e.reciprocal(o, i_), [sk], [sk])
        p.tt("dve", tmp4, Le, og.unsqueeze(3).to_broadcast([128, TB, 4, 8]), ALU.mult, [lk, sk], [sk])
        p.op("dve", lambda e, o=ing, i_=tmp4.rearrange("p t g e -> p t e g"): e.reduce_sum(o, i_, AX.X), [sk], [sk])
        p.op("dve", lambda e, o=v1, i_=ing: e.reduce_max(o, i_, AX.X), [sk], [sk])
        p.tt("dve", oh1, ing, bc3(v1, 8), ALU.is_equal, [sk], [sk])
        p.stt("dve", ing2, oh1, -1e30, ing, ALU.mult, ALU.add, [sk], [sk])
        p.op("dve", lambda e, o=v2, i_=ing2: e.reduce_max(o, i_, AX.X), [sk], [sk])
        p.tt("dve", oh2, ing2, bc3(v2, 8), ALU.is_equal, [sk], [sk])
        p.tt("dve", dd, v1, v2, ALU.subtract, [sk], [sk])
        p.act(p1, dd, AF.Sigmoid, [sk], [sk])
        p.tt("dve", info[:, i0:i0 + TB, 0, 1], gp, p1, ALU.mult, [sk, "info"], ["info"])
        p.tt("dve", info[:, i0:i0 + TB, 1, 1], gp, info[:, i0:i0 + TB, 0, 1], ALU.subtract, [sk, "info"], ["info"])
        ogb = og.unsqueeze(3).to_broadcast([128, TB, 4, 8])
        E1b = E1[:, i0:i0 + TB, :].rearrange("p t (g e) -> p t g e", g=4)
        E2b = E2[:, i0:i0 + TB, :].rearrange("p t (g e) -> p t g e", g=4)
        ek = [("E1", i0 + t) for t in range(TB)] + [("E2", i0 + t) for t in range(TB)]
        p.tt("dve", E1b, ogb, oh1.unsqueeze(2).to_broadcast([128, TB, 4, 8]), ALU.mult, [sk], ek[:TB])
        p.tt("dve", E2b, ogb, oh2.unsqueeze(2).to_broadcast([128, TB, 4, 8]), ALU.mult, [sk], ek[TB:])
        p.tt("pool", Mb[b].rearrange("p (t e) -> p t e", t=TB), E1[:, i0:i0 + TB, :], E2[:, i0:i0 + TB, :], ALU.add, ek, [("Mb", b)])
        for t in range(TB):
            p.mm(psR[b][:, t * 64:t * 64 + 32], Ls, Mb[b][:, t * 32:(t + 1) * 32], True, True, ["Ls", ("Mb", b)], [("psR", b)], sig=False)
            p.mm(psR[b][:, t * 64 + 32:t * 64 + 64], onesb, Mb[b][:, t * 32:(t + 1) * 32], True, True, ["onesb", ("Mb", b)], [("psR", b)],
                 sig=(t == TB - 1))
        for t in range(TB):
            p.tt("dve", RK[:, i0 + t, :], psR[b][:, t * 64:t * 64 + 32], carry, ALU.add, [("psR", b), "carry"], [("RK", i0 + t)])
            p.tt("dve", carry, carry, psR[b][:, t * 64 + 32:t * 64 + 64], ALU.add, [("psR", b), "carry"], ["carry"])
    if MOE_STOP == 1:
        return
    p.barrier()
    mem.reset(keep)
    cnt_i = mem.f32(32).bitcast(I32)
    padded = mem.f32(32)
    pend = mem.f32(32)
    pstart = mem.f32(32)
    ones32 = mem.f32(32)
    bst = mem.f32(NB)
    be = mem.f32(NB)
    need = mem.f32(NB)
    big3 = mem.f32(max(NB, NT) * 32)
    zer = mem.f32(NB * 2)
    p.dma(bst, c.cd["bstart"][:, 0:NB], writes=["bst"])
    p.op("pool", lambda e: e.memset(ones32, 1.0), [], ["ones32"])
    p.op("pool", lambda e: e.memset(zer, 0.0), [], ["zer"])
    p.dma(c.rowinfo.rearrange("(p a) b -> p (a b)", p=128), zer, reads=["zer"], writes=["rowinfo"])
    p.ts("dve", cnt_i, carry, 127.0, None, ALU.add, None, ["carry"], ["cnt_i"])
    p.ts("dve", cnt_i, cnt_i, 7, None, ALU.arith_shift_right, None, ["cnt_i"], ["cnt_i"])
    p.ts("dve", cnt_i, cnt_i, 7, None, ALU.logical_shift_left, None, ["cnt_i"], ["cnt_i"])
    p.cp("dve", padded, cnt_i, ["cnt_i"], ["padded"])
    p.op("dve", lambda e: e.tensor_tensor_scan(pend, ones32, padded, 0.0, ALU.mult, ALU.add), ["padded", "ones32"], ["pend"])
    p.tt("dve", pstart, pend, padded, ALU.subtract, ["pend", "padded"], ["pstart"])
    cmp3 = big3[:, 0:NB * 32].rearrange("p (b e) -> p b e", e=32)
    p.tt("dve", cmp3, bst.unsqueeze(2).to_broadcast([128, NB, 32]), pend.unsqueeze(1).to_broadcast([128, NB, 32]), ALU.is_ge,
         ["bst", "pend"], ["big3"])
    p.op("dve", lambda e: e.reduce_sum(be, cmp3, AX.X), ["big3"], ["be"])
    p.ts("dve", be, be, float(NE - 1), None, ALU.min, None, ["be"], ["be"])
    p.ts("dve", be, be, 128.0, float(0 if c.precast else l * NE * 128), ALU.mult, ALU.add, ["be"], ["be"])
    p.op("dve", lambda e: e.memset(need, 1.0), [], ["need"])
    if not ALWAYS_LOAD:
        p.tt("dve", need[:, 4:NB], be[:, 4:NB], be[:, 0:NB - 4], ALU.not_equal, ["be", "need"], ["need"])
    p.ts("dve", be, be, pcol, -OOB_IDX, ALU.add, ALU.add, ["be", "pcol"], ["be"])
    p.tt("dve", be, be, need, ALU.mult, ["be", "need"], ["be"])
    p.ts("dve", widx, be, OOB_IDX, None, ALU.add, None, ["be"], ["widx"])
    pr3 = big3[:, 0:NT * 32].rearrange("p (i e) -> p i e", e=32)
    p.tt("dve", RK, RK, pstart.unsqueeze(1).to_broadcast([128, NT, 32]), ALU.add, [("RK", i) for i in range(NT)] + ["pstart"], ["RKall"])
    for k, E in enumerate((E1, E2)):
        p.tt("dve", pr3, E, RK, ALU.mult, ["RKall", "big3"] + [(("E1", "E2")[k], i) for i in range(NT)], ["big3"])
        p.op("dve", lambda e: e.reduce_sum(destf, pr3, AX.X), ["big3"], ["destf"])
        p.cp("dve", dest[k], destf, ["destf"], [("dest", k)])
    for i in range(NT):
        for k in range(2):
            p.idma(c.rowinfo, IOA(dest[k][:, i:i + 1], 0), info[:, i, k, :], None,
                   reads=[("dest", k), "info", "rowinfo"], writes=[("risc", i, k)])
    p.barrier()
    if MOE_STOP == 2:
        return
    mem.reset(keep)
    NW, NX = 4, 6
    wg = [mem.bf16(8 * 512) for _ in range(NW)]
    wu = [mem.bf16(8 * 512) for _ in range(NW)]
    wd = [mem.bf16(4 * 1024) for _ in range(NW)]
    riall = mem.f32(NB * 2).rearrange("p (b a) -> p b a", a=2)
    xg = [mem.f32(1024) for _ in range(NX)]
    xT = [mem.bf16(8 * 128).rearrange("p (j t) -> p j t", j=8) for _ in range(3)]
    sg = [mem.f32(512) for _ in range(2)]
    hT = [mem.bf16(512).rearrange("p (j t) -> p j t", j=4) for _ in range(2)]
    yb = [mem.f32(1024) for _ in range(2)]
    if c.precast:
        wgv, wuv, wdv = c.wgb[l], c.wub[l], c.wdb[l]
    else:
        wgv = c.w_gate.rearrange("l e (p j) f -> (l e p) (j f)", j=8)
        wuv = c.w_up.rearrange("l e (p j) f -> (l e p) (j f)", j=8)
        wdv = c.w_down.rearrange("l e (p j) d -> (l e p) (j d)", j=4)
    psG, psU, psY = [c.ps[2], c.ps[3]], [c.ps[4], c.ps[5]], [c.ps[6], c.ps[7]]
    WB = (NE if c.precast else DEPTH * NE) * 128 - 1
    p.dma(riall, c.rowinfo.rearrange("(b p) a -> p b a", p=128), writes=["riall"])

    def stEx(bk):
        x = bk % NX
        p.idma(xg[x], None, xin, IOA(riall.bitcast(I32)[:, bk, 0:1], 0), reads=["riall"], writes=[("xg", x)])

    def stE0(bk):
        w = bk % NW
        wi = IOA(widx[:, bk:bk + 1], 0)
        p.idma(wg[w], None, wgv, wi, reads=["widx"], writes=[("wg", w)], bounds=WB)
        p.idma(wu[w], None, wuv, wi, reads=["widx"], writes=[("wu", w)], bounds=WB)
        p.idma(wd[w], None, wdv, wi, reads=["widx"], writes=[("wd", w)], bounds=WB)

    def stE1a(bk):
        b3 = bk % 3
        x = bk % NX
        xg3 = xg[x].rearrange("p (q j) -> p j q", j=8)
        for half in range(2):
            for j in range(4):
                p.tr(psX[half][:, j * 128:(j + 1) * 128], xg3[:, half * 4 + j, :], c.ident, [("xg", x), "ident"], [("psX", half)], sig=(j == 3))
            p.cp("act" if half else "dve", xT[b3][:, half * 4:(half + 1) * 4, :], psX[half][:, :].rearrange("p (j t) -> p j t", j=4),
                 [("psX", half)], [("xT", b3)])

    def stE1(bk):
        b = bk % 2
        b3 = bk % 3
        w = bk % NW
        wg4 = wg[w].rearrange("p (j q r) -> p j r q", j=8, r=4)
        wu4 = wu[w].rearrange("p (j q r) -> p j r q", j=8, r=4)
        for (w4, ps_, wk, pk) in ((wg4, psG[b], ("wg", w), ("psG", b)), (wu4, psU[b], ("wu", w), ("psU", b))):
            for fj in range(4):
                for j in range(8):
                    p.mm(ps_[:, fj * 128:(fj + 1) * 128], w4[:, j, fj, :], xT[b3][:, j, :], j == 0, j == 7, [wk, ("xT", b3)], [pk],
                         sig=(j == 7 and fj == 3))
        p.act(sg[b], psG[b][:, :], AF.Silu, [("psG", b)], [("sg", b)])
        p.tt("dve", hT[b].rearrange("p j t -> p (j t)"), sg[b], psU[b][:, :], ALU.mult, [("sg", b), ("psU", b)], [("hT", b)])

    def stE2(bk):
        b = bk % 2
        w = bk % NW
        wd3 = wd[w].rearrange("p (j d) -> p j d", j=4)
        gate = riall[:, bk, 1:2]
        for half in range(2):
            for fj in range(4):
                p.mm(psY[half][:, :], hT[b][:, fj, :], wd3[:, fj, half * 512:(half + 1) * 512], fj == 0, fj == 3,
                     [("hT", b), ("wd", w)], [("psY", half)])
            if half == 0:
                p.act(yb[b][:, 0:512], psY[0][:, :], AF.Copy, [("psY", 0), "riall"], [("yb", b)], scale=gate)
            else:
                p.ts("dve", yb[b][:, 512:1024], psY[1][:, :], gate, None, ALU.mult, None, [("psY", 1), "riall"], [("yb", b)])
        p.dma(c.yrows[bk * 128:(bk + 1) * 128, :], yb[b], reads=[("yb", b)])

    XL = NX - 1
    for bk in range(min(XL, NB)):
        stEx(bk)
    for bk in range(min(NW, NB)):
        stE0(bk)
    stE1a(0)
    stE1a(1)
    stE1(0)
    for bk in range(NB):
        if bk + 2 < NB:
            stE1a(bk + 2)
        if bk + 1 < NB:
            stE1(bk + 1)
        stE2(bk)
        if bk + NW < NB:
            stE0(bk + NW)
        if bk + XL < NB:
            stEx(bk + XL)
    p.barrier()
    if MOE_STOP == 3:
        return
    mem.reset(keep)
    NY = 4
    y1 = [mem.f32(1024) for _ in range(NY)]
    y2 = [mem.f32(1024) for _ in range(NY)]
    xt = [mem.f32(1024) for _ in range(NY)]
    hh = [mem.f32(1024) for _ in range(2)]
    xo = [mem.f32(1024) for _ in range(2)]
    c.ln_st = [mem.f32(12) for _ in range(2)]
    c.ln_mv = [mem.f32(4) for _ in range(2)]

    def stC1(i):
        yb_ = i % NY
        p.idma(y1[yb_], None, c.yrows, IOA(dest[0][:, i:i + 1], 0), reads=[("dest", 0)], writes=[("y1", yb_)])
        p.idma(y2[yb_], None, c.yrows, IOA(dest[1][:, i:i + 1], 0), reads=[("dest", 1)], writes=[("y2", yb_)])
        p.dma(xt[yb_], xin[i * 128:(i + 1) * 128, :], writes=[("xt", yb_)])

    def stC2(i):
        b = i % 2
        yb_ = i % NY
        rows = slice(i * 128, (i + 1) * 128)
        p.stt("dve", hh[b], xt[yb_], float(ALPHA), y1[yb_], ALU.mult, ALU.add, [("xt", yb_), ("y1", yb_)], [("hh", b)])
        p.tt("pool", hh[b], hh[b], y2[yb_], ALU.add, [("hh", b), ("y2", yb_)], [("hh", b)])
        layer_norm_tile(c, hh[b], xo[b], G, Bt, b, eps)
        p.dma(xout[rows, :], xo[b], reads=[("lnout", b)], q="pool")

    for i in range(min(2, NT)):
        stC1(i)
    for i in range(NT):
        if i + 2 < NT:
            stC1(i + 2)
        stC2(i)


def make_consts(T):
    half = HD // 2
    inv = 1.0 / (10000.0 ** np.linspace(0.0, 1.0, half, dtype=np.float32))
    pos = np.arange(T, dtype=np.float32)
    ang = (pos[None, :] * inv[:, None]).astype(np.float32)
    cos = np.cos(ang).astype(np.float32)
    sin = np.sin(ang).astype(np.float32)
    cosT = np.concatenate([cos, cos], 0)
    sinT = np.concatenate([-sin, sin], 0)
    ident = np.eye(128, dtype=np.float32)
    tri64 = np.triu(np.ones((64, 64), np.float32))
    tri128 = np.triu(np.ones((128, 128), np.float32))
    SPm = min(T, 1024)
    rm = np.ones((SPm,), np.float32)
    rm[::64] = 0.0
    rmask = np.broadcast_to(rm[None], (128, SPm)).copy()
    lg = np.log(1.0 - np.power(2.0, -5.0 - np.arange(NH, dtype=np.float64)))
    i = np.arange(64, dtype=np.float64)
    sc = HD ** -0.5
    rdec = np.stack([np.exp(lg[:, None] * (i + 1)), sc * np.exp(-lg[:, None] * (i + 1)),
                     sc * np.exp(lg[:, None] * (63 - i))], 0)
    rdec = np.broadcast_to(rdec.reshape(1, -1), (128, 3 * 4 * 64)).astype(np.float32).copy()
    rel = np.broadcast_to(np.exp(lg * 64)[None], (128, 4)).astype(np.float32).copy()
    ones128 = np.full((128, 128), 1.0 / 128, np.float32)
    ones1 = np.ones((128, 128), np.float32)
    sel4 = np.zeros((4, 4, 128), np.float32)
    for h in range(4):
        sel4[h, h, :] = 1.0
    sel4 = sel4.reshape(4, 512)
    ntri128 = ((1.0 - tri128) * -30000.0).astype(np.float32)
    lstrict = np.triu(np.ones((128, 128), np.float32), 1)
    NTmax = 64
    tokid = (np.arange(NTmax, dtype=np.int32)[None, :] * 128 + np.arange(128, dtype=np.int32)[:, None]).astype(np.int32)
    bstart = np.broadcast_to((np.arange(2 * NTmax + NE, dtype=np.float32) * 128)[None], (128, 2 * NTmax + NE)).copy()
    pcol = np.arange(128, dtype=np.float32).reshape(128, 1)
    return {"lstrict": lstrict, "tokid": tokid, "bstart": bstart, "pcol": pcol, "ones1": ones1, "sel4": sel4, "ntri128": ntri128, "cosT": cosT, "sinT": sinT, "ident": ident, "tri64": tri64, "tri128": tri128, "rmask": rmask,
            "rdec": rdec, "rel": rel, "ones128": ones128}


CONST_SHAPES = lambda T: {"cosT": [128, T], "sinT": [128, T], "ident": [128, 128], "tri64": [64, 64],
                          "tri128": [128, 128], "rmask": [128, min(T, 1024)], "rdec": [128, 768], "rel": [128, 4],
                          "ones128": [128, 128], "ones1": [128, 128], "sel4": [4, 512], "ntri128": [128, 128],
                          "lstrict": [128, 128], "tokid": [128, 64], "bstart": [128, 160], "pcol": [128, 1]}


def build_program(NSEQ, T, phases=None, debug=False):
    p = Prog()
    nc = p.nc
    c = Ctx()
    c.p, c.T, c.NSEQ = p, T, NSEQ
    c.precast = PRECAST
    c.dbg = phases is not None
    NTOK = NSEQ * T
    c.x = nc.dram_tensor("x", [NTOK, D], F32, kind="ExternalInput").ap()
    c.w_in = nc.dram_tensor("w_in", [DEPTH, D, D_IN], F32, kind="ExternalInput").ap()
    c.lbl = nc.dram_tensor("hgrn_lb_logits", [DEPTH, MW], F32, kind="ExternalInput").ap()
    c.fox_bias = nc.dram_tensor("fox_fgate_bias", [DEPTH, NH], F32, kind="ExternalInput").ap()
    c.w_branch = nc.dram_tensor("w_branch", [DEPTH, 3, MW, D], F32, kind="ExternalInput").ap()
    c.w_out = nc.dram_tensor("w_out", [DEPTH, D, D], F32, kind="ExternalInput").ap()
    c.ln1_g = nc.dram_tensor("ln1_g", [DEPTH, D], F32, kind="ExternalInput").ap()
    c.ln1_b = nc.dram_tensor("ln1_b", [DEPTH, D], F32, kind="ExternalInput").ap()
    c.xmid = nc.dram_tensor("xmid", [NTOK, D], F32, kind=("ExternalOutput" if debug else "Internal")).ap()
    c.cd = {k: nc.dram_tensor(k, shp, I32 if k == "tokid" else F32, kind="ExternalInput").ap() for k, shp in CONST_SHAPES(T).items()}
    c.w_rg = nc.dram_tensor("w_router_group", [DEPTH, D, 4], F32, kind="ExternalInput").ap()
    c.w_re = nc.dram_tensor("w_router_expert", [DEPTH, D, NE], F32, kind="ExternalInput").ap()
    c.w_up = nc.dram_tensor("w_up", [DEPTH, NE, D, DFF], F32, kind="ExternalInput").ap()
    c.w_gate = nc.dram_tensor("w_gate", [DEPTH, NE, D, DFF], F32, kind="ExternalInput").ap()
    c.w_down = nc.dram_tensor("w_down", [DEPTH, NE, DFF, D], F32, kind="ExternalInput").ap()
    c.ln2_g = nc.dram_tensor("ln2_g", [DEPTH, D], F32, kind="ExternalInput").ap()
    c.ln2_b = nc.dram_tensor("ln2_b", [DEPTH, D], F32, kind="ExternalInput").ap()
    NBLK = 2 * (NTOK // 128) + NE
    c.rowinfo = nc.dram_tensor("rowinfo", [NBLK * 128, 2], F32, kind="Internal").ap()
    c.yrows = nc.dram_tensor("yrows", [NBLK * 128, D], F32, kind="Internal").ap()
    c.out = nc.dram_tensor("out", [NTOK, D], F32, kind="ExternalOutput").ap()
    c.wgb = [nc.dram_tensor("wgb%d" % l_, [NE * 128, 8 * DFF], BF16, kind="Internal").ap() for l_ in range(DEPTH)]
    c.wub = [nc.dram_tensor("wub%d" % l_, [NE * 128, 8 * DFF], BF16, kind="Internal").ap() for l_ in range(DEPTH)]
    c.wdb = [nc.dram_tensor("wdb%d" % l_, [NE * 128, 4 * D], BF16, kind="Internal").ap() for l_ in range(DEPTH)]
    kind = "ExternalOutput" if debug else "Internal"
    c.FM = [nc.dram_tensor("FM", [FM_ROWS, NTOK], F32, kind=kind).ap()]
    c.TM = nc.dram_tensor("TM", [NTOK, 1536], F32, kind=kind).ap()
    c.YT = nc.dram_tensor("YT", [1536, NTOK], BF16, kind=kind).ap()
    c.lb_d = [nc.dram_tensor("lb%d" % l, [128, 4], F32, kind="Internal").ap() for l in range(DEPTH)]
    c.mem = Mem(p)
    mem = c.mem
    c.ps = [p.raw_psum("ps%d" % i, [128, 512], F32) for i in range(8)]
    c.ident = mem.f32(128)
    c.identb = mem.bf16(128)
    c.epsb = mem.f32(1)
    c.oneb = mem.f32(1)
    mem.base = mem.off
    p.dma(c.ident, c.cd["ident"], writes=["ident"])
    p.dma(c.identb, c.cd["ident"], writes=["identb"], q="pool")
    p.op("pool", lambda e: e.memset(c.epsb, HN_EPS), [], ["epsb"])
    p.op("pool", lambda e: e.memset(c.oneb, 1.0), [], ["oneb"])
    lt = mem.f32(8)
    p.dma(lt[:, 0:4], c.lbl[0].rearrange("(h p) -> p h", p=128), writes=["lt"], allow_slow_non_contiguous=True)
    p.dma(lt[:, 4:8], c.lbl[1].rearrange("(h p) -> p h", p=128), writes=["lt"], allow_slow_non_contiguous=True)
    p.tt("dve", lt[:, 4:8], lt[:, 4:8], lt[:, 0:4], ALU.subtract, ["lt"], ["lt"])
    p.act(lt[:, 4:8], lt[:, 4:8], AF.Sigmoid, ["lt"], ["lt"])
    p.op("dve", lambda e: e.memset(lt[:, 0:4], 0.0), ["lt"], ["lt"])
    p.dma(c.lb_d[0], lt[:, 0:4], reads=["lt"])
    p.dma(c.lb_d[1], lt[:, 4:8], reads=["lt"])
    if phases is None:
        xl1 = nc.dram_tensor("xl1", [NTOK, D], F32, kind="Internal").ap()
        for l in range(DEPTH):
            xin = c.x if l == 0 else xl1
            xout = c.out if l == DEPTH - 1 else xl1
            phase_inproj(c, l, xin)
            phase_rec(c, l, "A")
            phase_fox(c, l)
            phase_rec(c, l, "C")
            phase_merge(c, l, xin, c.xmid)
            phase_moe(c, l, c.xmid, xout)
        return p.emit(), p
    L = c.dbg_layer = 0
    for ph in phases:
        if ph == "inproj":
            phase_inproj(c, L, c.x)
        elif ph == "recA":
            phase_rec(c, L, "A")
        elif ph == "recC":
            phase_rec(c, L, "C")
        elif ph == "fox":
            phase_fox(c, L)
        elif ph == "merge":
            phase_merge(c, L, c.x, c.xmid)
        elif ph == "moe":
            phase_moe(c, L, c.xmid, c.out)
    return p.emit(), p


_PROG_CACHE = {}


def kernel(x, w_in, w_branch, w_out, fox_fgate_bias, hgrn_lb_logits, ln1_g, ln1_b,
           w_router_group, w_router_expert, w_up, w_gate, w_down, ln2_g, ln2_b):
    x = np.asarray(x, dtype=np.float32)
    Bsz, T, Dm = x.shape
    NSEQ = Bsz // N_CORES
    key = (NSEQ, T)
    if key not in _PROG_CACHE:
        _PROG_CACHE[key] = build_program(NSEQ, T, phases=None, debug=False)[0]
    nc = _PROG_CACHE[key]
    f = lambda a: np.ascontiguousarray(np.asarray(a, dtype=np.float32))
    shared = {
        "w_in": f(w_in), "w_branch": f(w_branch), "w_out": f(w_out), "fox_fgate_bias": f(fox_fgate_bias),
        "hgrn_lb_logits": f(hgrn_lb_logits), "ln1_g": f(ln1_g), "ln1_b": f(ln1_b),
        "w_router_group": f(w_router_group), "w_router_expert": f(w_router_expert), "w_up": f(w_up),
        "w_gate": f(w_gate), "w_down": f(w_down), "ln2_g": f(ln2_g), "ln2_b": f(ln2_b),
    }
    shared.update(make_consts(T))
    in_maps = []
    for i in range(N_CORES):
        m = dict(shared)
        m["x"] = np.ascontiguousarray(x[i * NSEQ:(i + 1) * NSEQ].reshape(NSEQ * T, Dm))
        in_maps.append(m)
    res = run_bass_kernel_spmd(nc, in_maps, core_ids=list(range(N_CORES)))
    outs = [np.asarray(r["out"], dtype=np.float32).reshape(NSEQ, T, Dm) for r in res.results]
    return np.concatenate(outs, axis=0)
```
